# Optimizing a Trainium2 kernel written in Bass

```python
import jax, jax.numpy as jnp
from jax import lax
import numpy as np

D_MODEL = 1024
BATCH = 8
SEQ = 2048
DEPTH = 1

MIX_DIM = D_MODEL
HEAD_DIM = 64
ATTN_DIM = MIX_DIM // 2
N_ATTN_HEADS = ATTN_DIM // HEAD_DIM
POOL_DIM = MIX_DIM - ATTN_DIM
POOL_WINDOWS = (2, 4, 8, 16)
N_POOL_GROUPS = len(POOL_WINDOWS)
POOL_GROUP_DIM = POOL_DIM // N_POOL_GROUPS
Q_BLOCK = 128
IN_DIM = POOL_DIM + 3 * ATTN_DIM + N_ATTN_HEADS
N_EXPERT_GROUPS = 4
EXPERTS_PER_GROUP = 8
TOP_K = 2
D_EXPERT = D_MODEL // 2
PLE_DIM = 256
EPS = 1e-6

kernel_name = "hybrid_pool_fox_hmoe_ple"


def rms_norm(x, g):
    xf = x.astype(jnp.float32)
    y = xf * lax.rsqrt(jnp.mean(xf * xf, axis=-1, keepdims=True) + EPS)
    return (y * g.astype(jnp.float32)).astype(x.dtype)


def causal_pool_mixer(u, w_pool, s_pool):
    B, S, _ = u.shape
    ug = u.reshape(B, S, N_POOL_GROUPS, POOL_GROUP_DIM)
    cs = jnp.cumsum(ug.astype(jnp.float32), axis=1)
    pos = jnp.arange(1, S + 1, dtype=jnp.float32)
    means = []
    for gi, w in enumerate(POOL_WINDOWS):
        c = cs[:, :, gi]
        lag = jnp.pad(c, ((0, 0), (w, 0), (0, 0)))[:, :S]
        cnt = jnp.minimum(pos, jnp.float32(w))[None, :, None]
        means.append((c - lag) / cnt)
    mean = jnp.stack(means, axis=2)
    d = (mean - ug.astype(jnp.float32)).astype(u.dtype)
    y = jnp.einsum('bsgc,gcd->bsgd', d, w_pool)
    return y.reshape(B, S, POOL_DIM) * s_pool


def forgetting_attention(q, k, v, log_f):
    B, S, H, Dh = q.shape
    c = jnp.cumsum(log_f.astype(jnp.float32), axis=1).transpose(0, 2, 1)
    qf = q.astype(jnp.float32).transpose(0, 2, 1, 3) * (Dh ** -0.5)
    kf = k.astype(jnp.float32).transpose(0, 2, 1, 3)
    vt = v.transpose(0, 2, 1, 3)
    tri = jnp.tril(jnp.ones((Q_BLOCK, Q_BLOCK), dtype=bool))
    outs = []
    for i in range(S // Q_BLOCK):
        q0, q1 = i * Q_BLOCK, (i + 1) * Q_BLOCK
        s = jnp.einsum('bhqd,bhkd->bhqk', qf[:, :, q0:q1], kf[:, :, :q1])
        s = s + (c[:, :, q0:q1, None] - c[:, :, None, :q1])
        mask = jnp.concatenate([jnp.ones((Q_BLOCK, q0), dtype=bool), tri], axis=1)
        s = jnp.where(mask, s, -jnp.inf)
        pr = jax.nn.softmax(s, axis=-1)
        outs.append(jnp.einsum('bhqk,bhkd->bhqd', pr.astype(v.dtype), vt[:, :, :q1]))
    o = jnp.concatenate(outs, axis=2)
    return o.transpose(0, 2, 1, 3).reshape(B, S, H * Dh)


def hierarchical_moe(m, w_grp, b_grp, w_rt, b_rt, w_e_gate, w_e_up, w_e_down):
    B, S, D = m.shape
    G, E = N_EXPERT_GROUPS, EXPERTS_PER_GROUP
    t = m.reshape(-1, D)
    grp_prob = jax.nn.softmax((t @ w_grp + b_grp).astype(jnp.float32), axis=-1)
    g_idx = jnp.argmax(grp_prob, axis=-1)
    g_w = jnp.max(grp_prob, axis=-1)
    ex_logits = (t @ w_rt + b_rt).astype(jnp.float32).reshape(-1, G, E)
    sel = jnp.take_along_axis(ex_logits, g_idx[:, None, None], axis=1)[:, 0]
    top_v, top_i = lax.top_k(sel, TOP_K)
    top_w = jax.nn.softmax(top_v, axis=-1) * g_w[:, None]
    e_comb = jnp.sum(jax.nn.one_hot(top_i, E, dtype=jnp.float32) * top_w[..., None], axis=1)
    comb = (jax.nn.one_hot(g_idx, G, dtype=jnp.float32)[:, :, None] * e_comb[:, None, :]).astype(t.dtype)
    y = jnp.zeros_like(t)
    for g in range(G):
        hg = jnp.einsum('nd,edf->nef', t, w_e_gate[g])
        hu = jnp.einsum('nd,edf->nef', t, w_e_up[g])
        a = jax.nn.silu(hg) * hu * comb[:, g, :, None]
        y = y + jnp.einsum('nef,efd->nd', a, w_e_down[g])
    return y.reshape(B, S, D)


def setup_inputs(seed: int = 0) -> dict:
    key = jax.random.key(seed)
    ks = jax.random.split(key, 24)
    L, D, H = DEPTH, D_MODEL, N_ATTN_HEADS
    G, E, F = N_EXPERT_GROUPS, EXPERTS_PER_GROUP, D_EXPERT
    nrm = lambda k, shape, fan_in: jax.random.normal(k, shape, jnp.float32) * (fan_in ** -0.5)
    gain = lambda k, shape: 1.0 + 0.02 * jax.random.normal(k, shape, jnp.float32)
    return {
        "x": jax.random.normal(ks[0], (BATCH, SEQ, D), jnp.float32),
        "p": jax.random.normal(ks[1], (L, BATCH, SEQ, PLE_DIM), jnp.float32),
        "g_mix": gain(ks[2], (L, D)),
        "w_in": nrm(ks[3], (L, D, IN_DIM), D),
        "b_f": jnp.linspace(1.0, 4.0, H, dtype=jnp.float32)[None, :] + 0.1 * jax.random.normal(ks[4], (L, H), jnp.float32),
        "w_pool": nrm(ks[5], (L, N_POOL_GROUPS, POOL_GROUP_DIM, POOL_GROUP_DIM), POOL_GROUP_DIM),
        "s_pool": gain(ks[6], (L, POOL_DIM)),
        "w_out": nrm(ks[7], (L, MIX_DIM, D), MIX_DIM),
        "g_ffn": gain(ks[8], (L, D)),
        "w_grp": nrm(ks[9], (L, D, G), D),
        "b_grp": 0.01 * jax.random.normal(ks[10], (L, G), jnp.float32),
        "w_rt": nrm(ks[11], (L, D, G * E), D),
        "b_rt": 0.01 * jax.random.normal(ks[12], (L, G * E), jnp.float32),
        "w_e_gate": nrm(ks[13], (L, G, E, D, F), D),
        "w_e_up": nrm(ks[14], (L, G, E, D, F), D),
        "w_e_down": nrm(ks[15], (L, G, E, F, D), F),
        "g_ple": gain(ks[16], (L, D)),
        "w_ple_gate": nrm(ks[17], (L, D, D), D),
        "w_ple_proj": nrm(ks[18], (L, PLE_DIM, D), PLE_DIM),
        "g_final": gain(ks[19], (D,)),
    }


def reference(x, p, g_mix, w_in, b_f, w_pool, s_pool, w_out, g_ffn, w_grp, b_grp,
              w_rt, b_rt, w_e_gate, w_e_up, w_e_down, g_ple, w_ple_gate, w_ple_proj, g_final):
    B, S, D = x.shape
    H = N_ATTN_HEADS
    h = x
    for i in range(DEPTH):
        a = rms_norm(h, g_mix[i])
        z = a @ w_in[i]
        o0 = POOL_DIM
        u_pool = z[..., :o0]
        q = z[..., o0:o0 + ATTN_DIM].reshape(B, S, H, HEAD_DIM)
        k = z[..., o0 + ATTN_DIM:o0 + 2 * ATTN_DIM].reshape(B, S, H, HEAD_DIM)
        v = z[..., o0 + 2 * ATTN_DIM:o0 + 3 * ATTN_DIM].reshape(B, S, H, HEAD_DIM)
        f_logit = z[..., o0 + 3 * ATTN_DIM:] + b_f[i]
        log_f = jax.nn.log_sigmoid(f_logit.astype(jnp.float32))
        pool_out = causal_pool_mixer(u_pool, w_pool[i], s_pool[i])
        attn_out = forgetting_attention(q, k, v, log_f).astype(h.dtype)
        h = h + jnp.concatenate([pool_out, attn_out], axis=-1) @ w_out[i]
        h = h + hierarchical_moe(rms_norm(h, g_ffn[i]), w_grp[i], b_grp[i], w_rt[i], b_rt[i],
                                 w_e_gate[i], w_e_up[i], w_e_down[i])
        gate = jax.nn.sigmoid(rms_norm(h, g_ple[i]) @ w_ple_gate[i])
        h = h + gate * (p[i] @ w_ple_proj[i])
    return rms_norm(h, g_final)
```

```python
import numpy as np
from contextlib import ExitStack
import concourse.bass as bass
import concourse.mybir as mybir
from concourse.bass_utils import run_bass_kernel_spmd

F32 = mybir.dt.float32
BF16 = mybir.dt.bfloat16
I32 = mybir.dt.int32
U32 = mybir.dt.uint32
AF = mybir.ActivationFunctionType
ALU = mybir.AluOpType
AX = mybir.AxisListType

S = 2048
D = 1024
NT = 16
NCH = 8
PLE = 256
EPS = 1e-6
N_CORES = 8
NEXP = 32
NOVF = 15
NSLOT = 32 + NOVF
NPRE = 28
PE_SKIP_SELF = True
OOB_SKIP = True
ARENA_BYTES = 210944


class Res:
    __slots__ = ("name", "w", "r")

    def __init__(self, name):
        self.name = name
        self.w = {}
        self.r = {}


class Eng:
    def __init__(self, name, obj, sem):
        self.name = name
        self.e = obj
        self.sem = sem
        self.cnt = 0
        self.waited = {}

    def wait_tok(self, s, v):
        if v > 0 and self.waited.get(s, 0) < v:
            self.e.wait_ge(s, v)
            self.waited[s] = v

    def sync(self, reads=(), writes=(), merge_ok=None):
        deps = {}

        def add(s, v):
            if deps.get(s, 0) < v:
                deps[s] = v
        for r in reads:
            for s, v in r.w.items():
                add(s, v)
        for w in writes:
            for s, v in w.w.items():
                if merge_ok is None or s not in merge_ok:
                    add(s, v)
            for s, v in w.r.items():
                add(s, v)
        for s, v in deps.items():
            if s is self.sem and self.name == "pe" and PE_SKIP_SELF:
                continue
            self.wait_tok(s, v)

    def done(self, ins, reads=(), writes=()):
        self.cnt += 1
        ins.then_inc(self.sem, 1)
        tok = (self.sem, self.cnt)
        for r in reads:
            if r.r.get(self.sem, 0) < self.cnt:
                r.r[self.sem] = self.cnt
        for w in writes:
            w.w = {self.sem: self.cnt}
            w.r = {}
        return tok

    def op(self, fn, reads=(), writes=()):
        self.sync(reads, writes)
        ins = fn(self.e)
        return self.done(ins, reads, writes)


class Kern:
    def __init__(self, nc, stack, n_hw=24, n_sw=24):
        self.nc = nc
        mk = lambda n: stack.enter_context(nc.semaphore(n))
        self.pe = Eng("pe", nc.tensor, mk("s_pe"))
        self.act = Eng("act", nc.scalar, mk("s_act"))
        self.dve = Eng("dve", nc.vector, mk("s_dve"))
        self.pool = Eng("pool", nc.gpsimd, mk("s_pool"))
        self.sp = Eng("sp", nc.sync, mk("s_sp"))
        self.engs = [self.pe, self.act, self.dve, self.pool, self.sp]
        self.hw = [[mk(f"s_hw{i}"), 0] for i in range(n_hw)]
        self.sw = [[mk(f"s_sw{i}"), 0] for i in range(n_sw)]
        self.hwi = 0
        self.swi = 0
        self.dma_sems = set(x[0] for x in self.hw) | set(x[0] for x in self.sw)

    def _slot(self, eng):
        if eng is self.pool:
            slot = self.sw[self.swi]
            self.swi = (self.swi + 1) % len(self.sw)
        else:
            slot = self.hw[self.hwi]
            self.hwi = (self.hwi + 1) % len(self.hw)
        return slot

    def _pre(self, eng, reads, writes, merge):
        slot = self._slot(eng)
        sem, cnt = slot
        eng.sync(reads, writes, merge_ok=self.dma_sems if merge else None)
        eng.wait_tok(sem, cnt)
        slot[1] = cnt + 16
        return sem, cnt + 16

    def _post(self, ins, sem, val, reads, writes, merge):
        ins.then_inc(sem, 16)
        for r in reads:
            if r.r.get(sem, 0) < val:
                r.r[sem] = val
        for w in writes:
            if merge:
                w.w[sem] = val
            else:
                w.w = {sem: val}
                w.r = {}
        return (sem, val)

    def dma(self, eng, out, in_, reads=(), writes=(), merge=False):
        sem, val = self._pre(eng, reads, writes, merge)
        ins = eng.e.dma_start(out=out, in_=in_)
        return self._post(ins, sem, val, reads, writes, merge)

    def scatter(self, out_dram, idx, in_sb, reads=(), writes=(), merge=True):
        sem, val = self._pre(self.pool, reads, writes, merge)
        ins = self.pool.e.indirect_dma_start(out=out_dram, out_offset=bass.IndirectOffsetOnAxis(ap=idx, axis=0),
                                             in_=in_sb, in_offset=None)
        return self._post(ins, sem, val, reads, writes, merge)

    def gather(self, out_sb, in_dram, idx, bound=None, reads=(), writes=(), merge=False):
        sem, val = self._pre(self.pool, reads, writes, merge)
        off = bass.IndirectOffsetOnAxis(ap=idx, axis=0)
        if bound is None:
            ins = self.pool.e.indirect_dma_start(out=out_sb, out_offset=None, in_=in_dram, in_offset=off)
        else:
            ins = self.pool.e.indirect_dma_start(out=out_sb, out_offset=None, in_=in_dram, in_offset=off,
                                                 bounds_check=bound, oob_is_err=False)
        return self._post(ins, sem, val, reads, writes, merge)

    def barrier(self, skip=()):
        for e in self.engs:
            for x in self.engs:
                if x is not e:
                    e.wait_tok(x.sem, x.cnt)
            for sem, cnt in self.hw + self.sw:
                if (sem, cnt) in skip:
                    continue
                e.wait_tok(sem, cnt)


def build_program(stage=99):
    nc = bass.Bass("TRN2", target_bir_lowering=False)
    dram = lambda n, shp, kind="ExternalInput": nc.dram_tensor(n, shp, F32, kind=kind).ap()
    x_d = dram("x", [S, D])
    p_d = dram("p", [S, PLE])
    g_mix_d = dram("g_mix", [1, D])
    w_in_d = dram("w_in", [D, 2056])
    b_f_d = dram("b_f", [8, 1])
    w_pool_d = dram("w_pool", [128, 4, 128])
    s_pool_d = dram("s_pool", [128, 4])
    w_out_d = dram("w_out", [D, D])
    g_ffn_d = dram("g_ffn", [1, D])
    w_r_d = dram("w_r", [D, 36])
    b_r_d = dram("b_r", [1, 36])
    w_eg_d = dram("w_e_gate", [NEXP * 256, 2048])
    w_eu_d = dram("w_e_up", [NEXP * 256, 2048])
    w_ed_d = dram("w_e_down", [NEXP * 256, 2048])
    g_ple_d = dram("g_ple", [1, D])
    w_pg_d = dram("w_ple_gate", [D, D])
    w_pp_d = dram("w_ple_proj", [PLE, D])
    g_fin_d = dram("g_final", [1, D])
    out_d = dram("out", [S, D], kind="ExternalOutput")

    with ExitStack() as st:
        K = Kern(nc, st)
        pe, act, dve, pool, sp = K.pe, K.act, K.dve, K.pool, K.sp
        arena = nc.alloc_sbuf_tensor("arena", [128, ARENA_BYTES // 4], F32)

        def view(off, nbytes, dt, pat=None, **kw):
            assert off % 4 == 0 and nbytes % 4 == 0 and off + nbytes <= ARENA_BYTES, (off, nbytes)
            a = arena[:, off // 4:(off + nbytes) // 4]
            if dt != F32:
                a = a.bitcast(dt)
            if pat:
                a = a.rearrange(pat, **kw)
            return a

        class Carver:
            def __init__(self, base, limit):
                self.o = base
                self.limit = limit

            def __call__(self, nbytes, dt, pat=None, **kw):
                nb = (nbytes + 63) // 64 * 64
                v = view(self.o, nb, dt)
                if nb != nbytes:
                    v = v[:, 0:nbytes // (2 if dt == BF16 else 4)]
                if pat:
                    v = v.rearrange(pat, **kw)
                self.o += nb
                assert self.o <= self.limit, (self.o, self.limit)
                return v

        _res = {}

        def R(name):
            if name not in _res:
                _res[name] = Res(name)
            return _res[name]

        ps = [st.enter_context(nc.psum_tensor(f"ps{i}", [128, 512], F32)) for i in range(8)]
        rps = [R(f"ps{i}") for i in range(8)]

        cp = Carver(0, ARENA_BYTES)
        H_OFF = cp.o
        H = cp(NT * D * 4, F32, "p (t d) -> p t d", t=NT)
        XT_OFF = cp.o
        XT = cp(NCH * S * 2, BF16, "p (c s) -> p c s", c=NCH)
        IDENT = cp(256, BF16)
        IDENTF = cp(512, F32)
        MASK = cp(256, BF16)
        MASKF = cp(512, F32)
        GG = [cp(D * 4, F32) for _ in range(2)]
        SSS = [cp(64, F32) for _ in range(2)]
        RSS = [cp(64, F32) for _ in range(2)]
        EPSC = cp(64, F32)
        INVC = cp(64, F32)
        ONEC = cp(64, F32)
        XN = [cp(D * 2, BF16) for _ in range(2)]
        JUNK = cp(D * 2, BF16)
        R2_OFF = cp.o
        rH = [R(f"H{t}") for t in range(NT)]
        rXT = [R(f"XT{b}") for b in range(4)]

        pool.op(lambda e: e.memset(IDENTF, 0.0), writes=[R("identf")])
        pool.op(lambda e: e.affine_select(out=IDENTF, in_=IDENTF, pattern=[[-1, 128]], compare_op=ALU.not_equal,
                                          fill=1.0, base=0, channel_multiplier=1),
                reads=[R("identf")], writes=[R("identf")])
        dve.op(lambda e: e.tensor_copy(out=IDENT, in_=IDENTF), reads=[R("identf")], writes=[R("ident")])
        pool.op(lambda e: e.memset(MASKF, 0.0), writes=[R("maskf")])
        pool.op(lambda e: e.affine_select(out=MASKF, in_=MASKF, pattern=[[1, 128]], compare_op=ALU.is_ge,
                                          fill=-30000.0, base=0, channel_multiplier=-1),
                reads=[R("maskf")], writes=[R("maskf")])
        dve.op(lambda e: e.tensor_copy(out=MASK, in_=MASKF), reads=[R("maskf")], writes=[R("mask")])
        dve.op(lambda e: e.memset(EPSC, EPS), writes=[R("epsc")])
        dve.op(lambda e: e.memset(ONEC, 1.0), writes=[R("onec")])
        for i in range(16):
            dve.op(lambda e, i=i: e.memset(INVC[:, i:i + 1], 1.0 / (i + 1)), writes=[R("invc")])

        bank_rr = [0]

        def next_bank(lo=0, hi=8):
            b = lo + bank_rr[0] % (hi - lo)
            bank_rr[0] += 1
            return b

        def pe_group(emit, reads, writes):
            pe.sync(reads, writes)
            last = emit(pe.e)
            return pe.done(last, reads, writes)

        def load_x(step=4):
            xv = x_d.rearrange("(t p) d -> p t d", p=128)
            for i in range(0, NT, step):
                K.dma(sp, H[:, i:i + step, :], xv[:, i:i + step, :], writes=rH[i:i + step])

        def load_gain(g_dram, k):
            K.dma(sp, GG[k], g_dram[0:1, :].partition_broadcast(128), writes=[R(f"G{k}")])

        def stats_group(k, g):
            for t in range(4 * g, 4 * g + 4):
                act.op(lambda e, t=t: e.activation(out=JUNK, in_=H[:, t, :], func=AF.Square, accum_out=SSS[k][:, t:t + 1]),
                       reads=[rH[t]], writes=[R(f"ss{k}_{g}")])
            act.op(lambda e: e.activation(out=RSS[k][:, 4 * g:4 * g + 4], in_=SSS[k][:, 4 * g:4 * g + 4], func=AF.Sqrt,
                                          scale=1.0 / D, bias=EPSC[:, 0:1]),
                   reads=[R(f"ss{k}_{g}"), R("epsc")], writes=[R(f"rs{k}_{g}")])
            dve.op(lambda e: e.reciprocal(out=RSS[k][:, 4 * g:4 * g + 4], in_=RSS[k][:, 4 * g:4 * g + 4]),
                   reads=[R(f"rs{k}_{g}")], writes=[R(f"rs{k}_{g}")])

        def stats(g_dram, k):
            load_gain(g_dram, k)
            for g in range(4):
                stats_group(k, g)

        def norm_tiles(k, g):
            for t in range(4 * g, 4 * g + 4):
                xn = XN[t % 2]
                rxn = R(f"xn{t % 2}")
                dve.op(lambda e, t=t, xn=xn: e.scalar_tensor_tensor(out=xn, in0=H[:, t, :], scalar=RSS[k][:, t:t + 1], in1=GG[k],
                                                                   op0=ALU.mult, op1=ALU.mult),
                       reads=[rH[t], R(f"rs{k}_{t // 4}"), R(f"G{k}")], writes=[rxn])
                b = 6 + t % 2
                psb = ps[b][:].bitcast(BF16)

                def tr(e, xn=xn, psb=psb):
                    last = None
                    for c in range(NCH):
                        last = e.transpose(psb[:, c * 128:(c + 1) * 128], xn[:, c * 128:(c + 1) * 128], IDENT)
                    return last
                pe_group(tr, [rxn, R("ident")], [rps[b]])
                act.op(lambda e, t=t, psb=psb: e.activation(out=XT[:, :, t * 128:(t + 1) * 128],
                                                            in_=psb.rearrange("p (c s) -> p c s", c=NCH), func=AF.Copy),
                       reads=[rps[b]], writes=[rXT[t // 4]])

        def norm_to_XT(g_dram, k):
            load_gain(g_dram, k)
            for g in range(4):
                stats_group(k, g)
                norm_tiles(k, g)

        def dump_H_and_finish():
            toks = []
            ov = out_d.rearrange("(t p) d -> p t d", p=128)
            for i in range(4):
                toks.append(K.dma(sp, ov[:, 4 * i:4 * i + 4, :], H[:, 4 * i:4 * i + 4, :], reads=rH[4 * i:4 * i + 4],
                                  writes=[R("out")]))
            for s_, v_ in toks:
                sp.wait_tok(s_, v_)

        load_x()
        c2_ = Carver(R2_OFF, ARENA_BYTES)
        WB = [c2_(NCH * 512 * 2, BF16, "p (c f) -> p c f", c=NCH) for _ in range(2)]
        WBF = c2_(NCH * 8 * 2, BF16, "p (c f) -> p c f", c=NCH)
        w_in_v = w_in_d.rearrange("(c p) f -> p c f", p=128)
        K.dma(pool, WBF, w_in_v[:, :, 2048:2056], writes=[R("wbf")])
        K.dma(pool, WB[0], w_in_v[:, :, 0:512], writes=[R("wb0")])
        K.dma(pool, WB[1], w_in_v[:, :, 512:1024], writes=[R("wb1")])
        ZT = view(ARENA_BYTES - D * 2, D * 2, BF16)
        dve.op(lambda e: e.memset(ZT, 0.0), writes=[R("zt")])
        XS_D = nc.dram_tensor("xs_d", [NSLOT * 256, D], BF16).ap()
        YS_D = nc.dram_tensor("ys_d", [NSLOT * 256, D], F32).ap()
        norm_to_XT(g_mix_d, 0)
        K.barrier()

        c1 = Carver(H_OFF, H_OFF + NT * D * 4)
        QT = c1(4 * S * 2, BF16, "p (c s) -> p c s", c=4)
        KT = c1(4 * S * 2, BF16, "p (c s) -> p c s", c=4)
        AUGQ_T = [c1(S * 2, BF16) for _ in range(3)]
        AUGK_T = [c1(S * 2, BF16) for _ in range(3)]
        AUGQ = [AUGQ_T[h // 3][32 * (h % 3):32 * (h % 3) + 6, :] for h in range(8)]
        AUGK = [AUGK_T[h // 3][32 * (h % 3):32 * (h % 3) + 6, :] for h in range(8)]
        PT = [c1(512 * 2, BF16) for _ in range(4)]
        OTOK = [c1(4 * 128 * 2, BF16, "p (i f) -> p i f", i=4) for _ in range(2)]
        REC = [c1(64, F32) for _ in range(2)]
        FIX = c1(64, F32)

        MIXTLO = c2_(4 * S * 2, BF16, "p (c s) -> p c s", c=4)
        PW_OFF = c2_.o
        U = c2_(S * 4, F32)
        PA = c2_(S * 4, F32)
        PB = c2_(S * 4, F32)
        DT = c2_(S * 2, BF16)
        PW_END = c2_.o
        RQ3 = c2_(3 * S * 2, BF16, "p (i s) -> p i s", i=3)
        V = c2_(NT * 8 * 65 * 2, BF16, "p (t h e) -> p t h e", t=NT, h=8)
        WP = c2_(4 * 128 * 2, BF16, "p (g d) -> p g d", g=4)
        SPOOL = c2_(64, F32)
        NBF = c2_(64, F32)

        K.dma(pool, WP, w_pool_d[:, :, :], writes=[R("wp")])
        K.dma(sp, SPOOL[:, 0:4], s_pool_d[:, :], writes=[R("spool")])
        K.dma(sp, NBF[0:8, 0:1], b_f_d[:, :], writes=[R("nbf")])
        dve.op(lambda e: e.tensor_scalar(out=NBF[0:8, 0:1], in0=NBF[0:8, 0:1], scalar1=-1.0, scalar2=None, op0=ALU.mult),
               reads=[R("nbf")], writes=[R("nbf")])

        def proj_fm(wb, rwb, col0, ncol, sb, bank, nrows=128):
            def emit(e):
                last = None
                for c in range(NCH):
                    last = e.matmul(ps[bank][0:nrows, :], lhsT=wb[:, c, col0:col0 + ncol],
                                    rhs=XT[:, c, sb * 512:(sb + 1) * 512], start=(c == 0), stop=(c == NCH - 1))
                return last
            pe_group(emit, [rwb, rXT[sb]], [rps[bank]])

        for sb in range(4):
            b = next_bank()
            proj_fm(WBF, R("wbf"), 0, 8, sb, b, nrows=8)
            act.op(lambda e, sb=sb, b=b: e.activation(out=PA[0:8, sb * 512:(sb + 1) * 512], in_=ps[b][0:8, :], func=AF.Exp,
                                                      scale=-1.0, bias=NBF[0:8, 0:1]),
                   reads=[rps[b], R("nbf")], writes=[R("PA")])
        act.op(lambda e: e.activation(out=PA[0:8, :], in_=PA[0:8, :], func=AF.Ln, scale=1.0, bias=ONEC[0:8, 0:1]),
               reads=[R("PA"), R("onec")], writes=[R("PA")])
        dve.op(lambda e: e.memset(U[0:8, :], 1.0), writes=[R("U")])
        dve.op(lambda e: e.tensor_tensor_scan(out=PB[0:8, :], data0=U[0:8, :], data1=PA[0:8, :], initial=0.0,
                                              op0=ALU.mult, op1=ALU.add),
               reads=[R("U"), R("PA")], writes=[R("PB")])
        dve.op(lambda e: e.tensor_scalar(out=RQ3[0:8, 0, :], in0=PB[0:8, :], scalar1=-1.0, scalar2=None, op0=ALU.mult),
               reads=[R("PB")], writes=[R("rq3")])
        dve.op(lambda e: e.scalar_tensor_tensor(out=PA[0:8, :], in0=PB[0:8, :], scalar=-1.0, in1=RQ3[0:8, 0, :],
                                                op0=ALU.mult, op1=ALU.subtract),
               reads=[R("PB"), R("rq3")], writes=[R("PA")])
        dve.op(lambda e: e.tensor_copy(out=RQ3[0:8, 1, :], in_=PA[0:8, :]), reads=[R("PA")], writes=[R("rq3")])
        dve.op(lambda e: e.tensor_tensor(out=PA[0:8, :], in0=PA[0:8, :], in1=RQ3[0:8, 1, :], op=ALU.subtract),
               reads=[R("PA"), R("rq3")], writes=[R("PA")])
        dve.op(lambda e: e.tensor_copy(out=RQ3[0:8, 2, :], in_=PA[0:8, :]), reads=[R("PA")], writes=[R("rq3")])

        for i in range(3):
            hs = [h for h in range(8) if h // 3 == i]
            pool.op(lambda e, i=i: e.memset(AUGQ_T[i], -1.0), writes=[R(f"augq{h}") for h in hs])
            pool.op(lambda e, i=i: e.memset(AUGK_T[i], 1.0), writes=[R(f"augk{h}") for h in hs])
        for h in range(8):
            for i in range(3):
                K.dma(sp, AUGQ[h][i:i + 1, :], RQ3[h:h + 1, i, :], reads=[R("rq3")], writes=[R(f"augq{h}")], merge=True)
                K.dma(sp, AUGK[h][3 + i:4 + i, :], RQ3[h:h + 1, i, :], reads=[R("rq3")], writes=[R(f"augk{h}")], merge=True)

        for s_ in range(NSLOT):
            K.dma(sp, XS_D[s_ * 256:(s_ + 1) * 256, :].rearrange("(j p) d -> p j d", p=128),
                  ZT.unsqueeze(1).to_broadcast([128, 2, D]), reads=[R("zt")], writes=[R("xs_zero")], merge=True)

        for g in range(4):
            for sb in range(4):
                b = next_bank()
                proj_fm(WB[0], R("wb0"), g * 128, 128, sb, b)
                act.op(lambda e, sb=sb, b=b: e.activation(out=U[:, sb * 512:(sb + 1) * 512], in_=ps[b][:, :], func=AF.Copy),
                       reads=[rps[b]], writes=[R("U")])
            w = 2 ** (g + 1)
            src, rsrc = U, R("U")
            bufs = [(PA, R("PA")), (PB, R("PB"))]
            for k in range(g + 1):
                sh = 2 ** k
                dst, rdst = bufs[k % 2]
                dve.op(lambda e, src=src, dst=dst, sh=sh: e.tensor_tensor(out=dst[:, sh:S], in0=src[:, sh:S], in1=src[:, 0:S - sh],
                                                                         op=ALU.add),
                       reads=[rsrc], writes=[rdst])
                dve.op(lambda e, src=src, dst=dst, sh=sh: e.tensor_copy(out=dst[:, 0:sh], in_=src[:, 0:sh]),
                       reads=[rsrc], writes=[rdst])
                src, rsrc = dst, rdst
            dve.op(lambda e, src=src, w=w: e.scalar_tensor_tensor(out=DT, in0=src, scalar=1.0 / w, in1=U, op0=ALU.mult,
                                                                 op1=ALU.subtract),
                   reads=[rsrc, R("U")], writes=[R("DT")])
            dve.op(lambda e, src=src, w=w: e.tensor_tensor(out=FIX[:, 0:w - 1], in0=src[:, 0:w - 1], in1=INVC[:, 0:w - 1],
                                                          op=ALU.mult),
                   reads=[rsrc, R("invc")], writes=[R("fix")])
            dve.op(lambda e, w=w: e.tensor_tensor(out=DT[:, 0:w - 1], in0=FIX[:, 0:w - 1], in1=U[:, 0:w - 1], op=ALU.subtract),
                   reads=[R("fix"), R("U")], writes=[R("DT")])
            for sb in range(4):
                b = next_bank()
                pe_group(lambda e, g=g, sb=sb, b=b: e.matmul(ps[b][:, :], lhsT=WP[:, g, :], rhs=DT[:, sb * 512:(sb + 1) * 512],
                                                             start=True, stop=True),
                         [R("wp"), R("DT")], [rps[b]])
                act.op(lambda e, g=g, sb=sb, b=b: e.activation(out=MIXTLO[:, g, sb * 512:(sb + 1) * 512], in_=ps[b][:, :],
                                                               func=AF.Copy, scale=SPOOL[:, g:g + 1]),
                       reads=[rps[b], R("spool")], writes=[R("mixtlo")])

        for c2 in range(4):
            for sb in range(4):
                b = next_bank()
                proj_fm(WB[1], R("wb1"), c2 * 128, 128, sb, b)
                act.op(lambda e, c2=c2, sb=sb, b=b: e.activation(out=QT[:, c2, sb * 512:(sb + 1) * 512], in_=ps[b][:, :],
                                                                 func=AF.Copy, scale=0.125),
                       reads=[rps[b]], writes=[R(f"qt{c2}")])
        K.dma(pool, WB[0], w_in_v[:, :, 1024:1536], writes=[R("wb0")])
        K.dma(pool, WB[1], w_in_v[:, :, 1536:2048], writes=[R("wb1")])
        WO = view(PW_OFF, NCH * D * 2, BF16, "p (c d) -> p c d", c=NCH)
        assert PW_OFF + NCH * D * 2 <= PW_END
        w_out_v = w_out_d.rearrange("(c p) d -> p c d", p=128)
        K.dma(pool, WO[:, 0:4, :], w_out_v[:, 0:4, :], writes=[R("wo0"), R("U")])
        K.dma(pool, WO[:, 4:8, :], w_out_v[:, 4:8, :], writes=[R("wo1"), R("PA")])
        WBF_D = {nm: nc.dram_tensor(f"wbf_{nm}", [max(NPRE, 1) * 256, 2048], BF16).ap() for nm in ("wg", "wu", "wd")}
        for e_ in range(NPRE):
            for (nm, wd) in (("wg", w_eg_d), ("wu", w_eu_d), ("wd", w_ed_d)):
                K.dma(pool, WBF_D[nm][e_ * 256:(e_ + 1) * 256, :].rearrange("(p h) f -> p h f", h=2),
                      wd[e_ * 256:(e_ + 1) * 256, :].rearrange("(p h) f -> p h f", h=2), writes=[R(f"wbf_{nm}{e_}")])
        for c2 in range(4):
            for sb in range(4):
                b = next_bank()
                proj_fm(WB[0], R("wb0"), c2 * 128, 128, sb, b)
                dve.op(lambda e, c2=c2, sb=sb, b=b: e.tensor_copy(out=KT[:, c2, sb * 512:(sb + 1) * 512], in_=ps[b][:, :]),
                       reads=[rps[b]], writes=[R(f"kt{c2}")])
        dve.op(lambda e: e.memset(V[:, :, :, 64:65], 1.0), writes=[R("V")])
        for t in range(NT):
            b = next_bank()

            def emit(e, t=t, b=b):
                last = None
                for c in range(NCH):
                    last = e.matmul(ps[b][:, :], lhsT=XT[:, c, t * 128:(t + 1) * 128], rhs=WB[1][:, c, :],
                                    start=(c == 0), stop=(c == NCH - 1))
                return last
            pe_group(emit, [R("wb1"), rXT[t // 4]], [rps[b]])
            src_v = ps[b][:, :].rearrange("p (h e) -> p h e", h=8)
            if t % 2 == 0:
                act.op(lambda e, t=t, src_v=src_v: e.activation(out=V[:, t, :, 0:64], in_=src_v, func=AF.Copy),
                       reads=[rps[b]], writes=[R("V")])
            else:
                dve.op(lambda e, t=t, src_v=src_v: e.tensor_copy(out=V[:, t, :, 0:64], in_=src_v),
                       reads=[rps[b]], writes=[R("V")])

        LA = 2
        items = []
        gidx = 0
        for c2 in range(4):
            for qb in range(4):
                for half in range(2):
                    nj = 4 * qb + 4
                    for j in range(nj):
                        items.append((c2, qb, half, j, gidx, j == nj - 1))
                    gidx += 1
        from collections import deque
        pend = deque()
        deferred = []
        cnt_s = [0]

        def emit_S(it):
            c2, qb, half, j, g, last = it
            hd = 2 * c2 + half
            base = 64 * half
            q_lo = max(qb * 512, j * 128)
            n = (qb + 1) * 512 - q_lo
            k = cnt_s[0]
            cnt_s[0] += 1
            sbk = 2 + k % 4
            pt = PT[k % 4]
            rpt = R(f"pt{k % 4}")
            diag = j >= 4 * qb

            def emit_s(e):
                e.matmul(ps[sbk][:, 0:n], lhsT=KT[base:base + 64, c2, j * 128:(j + 1) * 128],
                         rhs=QT[base:base + 64, c2, q_lo:q_lo + n], start=True, stop=False)
                last_ = e.matmul(ps[sbk][:, 0:n], lhsT=AUGK[hd][:, j * 128:(j + 1) * 128],
                                 rhs=AUGQ[hd][:, q_lo:q_lo + n], start=False, stop=not diag)
                if diag:
                    last_ = e.matmul(ps[sbk][:, 0:128], lhsT=IDENT, rhs=MASK, start=False, stop=True)
                return last_
            pe_group(emit_s, [R(f"kt{c2}"), R(f"qt{c2}"), R(f"augk{hd}"), R(f"augq{hd}"), R("mask"), R("ident")], [rps[sbk]])
            act.op(lambda e: e.activation(out=pt[:, 0:n], in_=ps[sbk][:, 0:n], func=AF.Exp), reads=[rps[sbk]], writes=[rpt])
            return (it, q_lo, pt, rpt)

        def emit_PV(rec):
            (c2, qb, half, j, g, last), q_lo, pt, rpt = rec
            h = 2 * c2 + half
            base = 64 * half
            ob = g % 2
            O = ps[ob][:, 0:260].rearrange("p (i e) -> p i e", e=65)

            def emit_o(e):
                last_ = None
                for i in range(max(j, 4 * qb), 4 * qb + 4):
                    off = i * 128 - q_lo
                    last_ = e.matmul(O[:, i - 4 * qb, :], lhsT=pt[:, off:off + 128], rhs=V[:, j, h, :],
                                     start=(j == 0 and i == 4 * qb), stop=(j == i), skip_group_check=True)
                return last_
            pe_group(emit_o, [rpt, R("V")], [rps[ob]])
            if not last:
                return
            ot = OTOK[qb % 2]
            rot = R(f"otok{qb % 2}")
            rec_ = REC[ob]
            rrec = R(f"rec{ob}")
            dve.op(lambda e: e.reciprocal(out=rec_[:, 0:4], in_=O[:, :, 64]), reads=[rps[ob]], writes=[rrec])
            dve.op(lambda e: e.tensor_tensor(out=ot[:, :, base:base + 64], in0=O[:, :, 0:64],
                                             in1=rec_[:, 0:4].unsqueeze(2).to_broadcast([128, 4, 64]), op=ALU.mult),
                   reads=[rps[ob], rrec], writes=[rot])
            if half == 1:
                def fin():
                    tb = 6 + qb % 2
                    psb = ps[tb][:].bitcast(BF16)

                    def tr(e):
                        last_ = None
                        for i in range(4):
                            last_ = e.transpose(psb[:, i * 128:(i + 1) * 128], ot[:, i, :], IDENT)
                        return last_
                    pe_group(tr, [rot, R("ident")], [rps[tb]])
                    act.op(lambda e: e.activation(out=XT[:, c2, qb * 512:(qb + 1) * 512], in_=psb[:, 0:512], func=AF.Copy),
                           reads=[rps[tb]], writes=[rXT[qb]])
                deferred.append([3, fin])

        def tick():
            for d_ in list(deferred):
                d_[0] -= 1
                if d_[0] <= 0:
                    deferred.remove(d_)
                    d_[1]()

        for it in items:
            pend.append(emit_S(it))
            if len(pend) > LA:
                emit_PV(pend.popleft())
            tick()
        while pend:
            emit_PV(pend.popleft())
            tick()
        while deferred:
            tick()

        K.barrier()
        load_x(step=1)
        for t in range(NT):
            for hf in range(2):
                b = next_bank()

                def emit(e, t=t, hf=hf, b=b):
                    last = None
                    for c in range(NCH):
                        lhsT = MIXTLO[:, c, t * 128:(t + 1) * 128] if c < 4 else XT[:, c - 4, t * 128:(t + 1) * 128]
                        last = e.matmul(ps[b][:, :], lhsT=lhsT, rhs=WO[:, c, hf * 512:(hf + 1) * 512],
                                        start=(c == 0), stop=(c == NCH - 1))
                    return last
                pe_group(emit, [R("mixtlo"), rXT[t // 4], R("wo0"), R("wo1")], [rps[b]])
                dve.op(lambda e, t=t, hf=hf, b=b: e.tensor_tensor(out=H[:, t, hf * 512:(hf + 1) * 512], in0=ps[b][:, :],
                                                                in1=H[:, t, hf * 512:(hf + 1) * 512], op=ALU.add),
                       reads=[rps[b], rH[t]], writes=[rH[t]])
        if stage == 1:
            dump_H_and_finish()
            return nc

        stats(g_ffn_d, 1)
        K.barrier()
        XNALL = XT.rearrange("p c s -> p (c s)").rearrange("p (t d) -> p t d", t=NT)
        rXN = [R(f"xnall{t}") for t in range(NT)]
        cb = Carver(R2_OFF, ARENA_BYTES)
        WG = [cb(4096 * 2, BF16) for _ in range(2)]
        WU = [cb(4096 * 2, BF16) for _ in range(2)]
        WD = [cb(4096 * 2, BF16) for _ in range(2)]
        W1 = cb(64, F32)
        W2 = cb(64, F32)
        IDX1 = cb(64, I32)
        IDX2 = cb(64, I32)
        IDXW = cb(2 * 16 * 4, I32)
        UNION_OFF = cb.o
        cr = Carver(UNION_OFF, ARENA_BYTES)
        XTT = [cr(NCH * 128 * 2, BF16, "p (c s) -> p c s", c=NCH) for _ in range(2)]
        WR = cr(NCH * 36 * 2, BF16, "p (c f) -> p c f", c=NCH)
        B36 = cr(36 * 4, F32)
        L = cr(NT * 36 * 4, F32, "p (t f) -> p t f", t=NT)
        GMAX, GW, M1, M2, ED, DR1, DR2, EIDO, OIDX = [cr(64, F32) for _ in range(9)]
        OH4 = cr(NT * 4 * 4, F32, "p (t g) -> p t g", t=NT)
        E4 = cr(NT * 4 * 4, F32, "p (t g) -> p t g", t=NT)
        SEL, SEL2, MK1, MK2 = [cr(NT * 8 * 4, F32, "p (t e) -> p t e", t=NT) for _ in range(4)]
        TMP, M321, M322, MSUM, POS, DEST = [cr(NT * 32 * 4, F32, "p (t g e) -> p t g e", t=NT, g=4) for _ in range(6)]
        MSUMB = cr(NT * 32 * 2, BF16, "p (t e) -> p t e", t=NT)
        TOT, NSO, OCI, OST, DELTA, BASEA, ONE32 = [cr(128, F32) for _ in range(7)]
        PIDX = cr(64, F32)
        IW = cr(2 * 16 * 4, F32)
        ONESB = cr(256, BF16)
        TRIB = cr(256, BF16)
        TRIF = cr(512, F32)
        cs = Carver(UNION_OFF, ARENA_BYTES)
        XSG = [cs(2 * D * 2, BF16, "p (j d) -> p j d", j=2) for _ in range(2)]
        XST = [cs(NCH * 256 * 2, BF16, "p (c s) -> p c s", c=NCH) for _ in range(2)]
        AT = [cs(4 * 256 * 2, BF16, "p (c s) -> p c s", c=4) for _ in range(2)]
        SG = [cs(256 * 2, BF16) for _ in range(2)]
        YSBS = [cs(2 * D * 4, F32, "p (j d) -> p j d", j=2) for _ in range(2)]
        WST = [cs(2048 * 4, F32)]
        cx = Carver(XT_OFF, XT_OFF + NCH * S * 2)
        WG.append(cx(4096 * 2, BF16))
        WU.append(cx(4096 * 2, BF16))
        WD.append(cx(4096 * 2, BF16))
        WST.append(cx(2048 * 4, F32))


        def wview(wd, e_):
            return wd.rearrange("(e p h) f -> e p h f", p=128, h=2)[e_]

        ORDER = []
        for i_ in range(16):
            ORDER += [2 * i_, 2 * i_ + 1] + ([32 + i_] if i_ < NOVF else [])
        assert sorted(ORDER) == list(range(NSLOT))

        def load_slot_weights_static(q_):
            s_ = ORDER[q_]
            wb = q_ % 3
            for (wt, wd, nm) in ((WG, w_eg_d, "wg"), (WU, w_eu_d, "wu"), (WD, w_ed_d, "wd")):
                if s_ < NPRE:
                    K.dma(pool, wt[wb].rearrange("p (h f) -> p h f", h=2), wview(WBF_D[nm], s_), reads=[R(f"wbf_{nm}{s_}")],
                          writes=[R(f"{nm}{wb}")])
                else:
                    K.dma(pool, wt[wb].rearrange("p (h f) -> p h f", h=2), wview(wd, s_), writes=[R(f"{nm}{wb}")])

        bound_reg = None
        if OOB_SKIP:
            bound_reg = nc.gpsimd.alloc_register("wbound")
            nc.gpsimd.reg_mov(bound_reg, NEXP * 256 - 1)

        dve.op(lambda e: e.memset(ONESB, 1.0), writes=[R("onesb")])
        pool.op(lambda e: e.memset(TRIF, 1.0), writes=[R("trif")])
        pool.op(lambda e: e.affine_select(out=TRIF, in_=TRIF, pattern=[[1, 128]], compare_op=ALU.is_ge, fill=0.0,
                                          base=-1, channel_multiplier=-1), reads=[R("trif")], writes=[R("trif")])
        dve.op(lambda e: e.tensor_copy(out=TRIB, in_=TRIF), reads=[R("trif")], writes=[R("trib")])
        for i in range(32):
            dve.op(lambda e, i=i: e.memset(BASEA[:, i:i + 1], 256.0 * i), writes=[R("basea")])
        for i in range(16):
            dve.op(lambda e, i=i: e.memset(OIDX[:, i:i + 1], float(i)), writes=[R("oidx")])
        dve.op(lambda e: e.memset(ONE32, 1.0), writes=[R("one32")])
        pe_group(lambda e: e.matmul(ps[5][:, 0:1], lhsT=TRIB, rhs=ONESB[:, 0:1], start=True, stop=True),
                 [R("trib"), R("onesb")], [rps[5]])
        dve.op(lambda e: e.tensor_copy(out=PIDX[:, 0:1], in_=ps[5][:, 0:1]), reads=[rps[5]], writes=[R("pidx")])

        K.dma(pool, WR, w_r_d.rearrange("(c p) f -> p c f", p=128), writes=[R("wr")])
        K.dma(sp, B36, b_r_d[0:1, :].partition_broadcast(128), writes=[R("b36")])
        load_slot_weights_static(0)
        load_slot_weights_static(1)

        def router(t):
            xtt = XTT[t % 2]
            rb = 4 + t % 2

            def emit(e):
                last = None
                for c in range(NCH):
                    last = e.matmul(ps[rb][:, 0:36], lhsT=xtt[:, c, :], rhs=WR[:, c, :], start=(c == 0), stop=(c == NCH - 1))
                return last
            pe_group(emit, [R("wr"), R(f"xtt{t % 2}")], [rps[rb]])
            dve.op(lambda e: e.tensor_tensor(out=L[:, t, :], in0=ps[rb][:, 0:36], in1=B36, op=ALU.add),
                   reads=[rps[rb], R("b36")], writes=[R("L")])

        for t in range(NT):
            dve.op(lambda e, t=t: e.scalar_tensor_tensor(out=XNALL[:, t, :], in0=H[:, t, :], scalar=RSS[1][:, t:t + 1], in1=GG[1],
                                                        op0=ALU.mult, op1=ALU.mult),
                   reads=[rH[t], R(f"rs1_{t // 4}"), R("G1")], writes=[rXN[t]])
            b = 6 + t % 2
            psb = ps[b][:].bitcast(BF16)

            def tr(e, t=t, psb=psb):
                last = None
                for c in range(NCH):
                    last = e.transpose(psb[:, c * 128:(c + 1) * 128], XNALL[:, t, c * 128:(c + 1) * 128], IDENT)
                return last
            pe_group(tr, [rXN[t], R("ident")], [rps[b]])
            act.op(lambda e, t=t, psb=psb: e.activation(out=XTT[t % 2], in_=psb.rearrange("p (c s) -> p c s", c=NCH), func=AF.Copy),
                   reads=[rps[b]], writes=[R(f"xtt{t % 2}")])
            if t >= 1:
                router(t - 1)
        router(NT - 1)

        rr = R("route")
        Lg = L[:, :, 0:4]
        Le = L[:, :, 4:36].rearrange("p t (g e) -> p t g e", g=4)
        bc = lambda a, shp, ax: a.unsqueeze(ax).to_broadcast(shp)
        V16 = lambda a: a[:, 0:NT]
        V32 = lambda a: a[:, 0:32]
        F3 = lambda a: a.rearrange("p t g e -> p t (g e)")
        croute = [R("L"), rr, R("basea"), R("oidx"), R("one32"), R("pidx")]
        dop = lambda fn: dve.op(fn, reads=croute, writes=[rr])
        dop(lambda e: e.tensor_reduce(out=V16(GMAX), in_=Lg, axis=AX.X, op=ALU.max))
        dop(lambda e: e.tensor_tensor(out=OH4, in0=Lg, in1=bc(V16(GMAX), [128, NT, 4], 2), op=ALU.is_equal))
        dop(lambda e: e.tensor_tensor(out=E4, in0=Lg, in1=bc(V16(GMAX), [128, NT, 4], 2), op=ALU.subtract))
        act.op(lambda e: e.activation(out=E4, in_=E4, func=AF.Exp), reads=[rr], writes=[rr])
        dop(lambda e: e.tensor_reduce(out=V16(GW), in_=E4, axis=AX.X, op=ALU.add))
        dop(lambda e: e.reciprocal(out=V16(GW), in_=V16(GW)))
        dop(lambda e: e.tensor_tensor(out=TMP, in0=Le, in1=bc(OH4, [128, NT, 4, 8], 3), op=ALU.mult))
        dop(lambda e: e.tensor_reduce(out=SEL, in_=TMP.rearrange("p t g e -> p t e g"), axis=AX.X, op=ALU.add))
        dop(lambda e: e.tensor_reduce(out=V16(M1), in_=SEL, axis=AX.X, op=ALU.max))
        dop(lambda e: e.tensor_tensor(out=MK1, in0=SEL, in1=bc(V16(M1), [128, NT, 8], 2), op=ALU.is_equal))
        dop(lambda e: e.scalar_tensor_tensor(out=SEL2, in0=MK1, scalar=-1e30, in1=SEL, op0=ALU.mult, op1=ALU.add))
        dop(lambda e: e.tensor_reduce(out=V16(M2), in_=SEL2, axis=AX.X, op=ALU.max))
        dop(lambda e: e.tensor_tensor(out=MK2, in0=SEL2, in1=bc(V16(M2), [128, NT, 8], 2), op=ALU.is_equal))
        dop(lambda e: e.tensor_tensor(out=V16(ED), in0=V16(M2), in1=V16(M1), op=ALU.subtract))
        act.op(lambda e: e.activation(out=V16(ED), in_=V16(ED), func=AF.Exp), reads=[rr], writes=[rr])
        dve.op(lambda e: e.tensor_scalar(out=V16(W1), in0=V16(ED), scalar1=1.0, scalar2=None, op0=ALU.add),
               reads=[rr], writes=[R("w12")])
        dve.op(lambda e: e.reciprocal(out=V16(W1), in_=V16(W1)), reads=[R("w12")], writes=[R("w12")])
        dve.op(lambda e: e.tensor_tensor(out=V16(W1), in0=V16(W1), in1=V16(GW), op=ALU.mult), reads=[R("w12"), rr], writes=[R("w12")])
        dve.op(lambda e: e.tensor_tensor(out=V16(W2), in0=V16(ED), in1=V16(W1), op=ALU.mult), reads=[R("w12"), rr], writes=[R("w12")])
        dop(lambda e: e.tensor_tensor(out=M321, in0=bc(OH4, [128, NT, 4, 8], 3), in1=bc(MK1, [128, NT, 4, 8], 2), op=ALU.mult))
        dop(lambda e: e.tensor_tensor(out=M322, in0=bc(OH4, [128, NT, 4, 8], 3), in1=bc(MK2, [128, NT, 4, 8], 2), op=ALU.mult))
        dop(lambda e: e.tensor_tensor(out=MSUM, in0=M321, in1=M322, op=ALU.add))
        dop(lambda e: e.tensor_copy(out=MSUMB, in_=F3(MSUM)))
        for t in range(NT):
            pb_ = 4 + t % 2

            def emit(e, t=t, pb_=pb_):
                for t2 in range(t):
                    e.matmul(ps[pb_][:, 0:32], lhsT=ONESB, rhs=MSUMB[:, t2, :], start=(t2 == 0), stop=False)
                return e.matmul(ps[pb_][:, 0:32], lhsT=TRIB, rhs=MSUMB[:, t, :], start=(t == 0), stop=True)
            pe_group(emit, [rr, R("onesb"), R("trib")], [rps[pb_]])
            dve.op(lambda e, t=t, pb_=pb_: e.tensor_copy(out=F3(POS)[:, t, :], in_=ps[pb_][:, 0:32]), reads=[rps[pb_]], writes=[R("pos")])

        def emit(e):
            last = None
            for t in range(NT):
                last = e.matmul(ps[5][:, 0:32], lhsT=ONESB, rhs=MSUMB[:, t, :], start=(t == 0), stop=(t == NT - 1))
            return last
        pe_group(emit, [rr, R("onesb")], [rps[5]])
        dve.op(lambda e: e.tensor_copy(out=V32(TOT), in_=ps[5][:, 0:32]), reads=[rps[5]], writes=[rr])
        croute.append(R("pos"))
        dop(lambda e: e.tensor_scalar(out=V32(NSO), in0=V32(TOT), scalar1=256.0, scalar2=None, op0=ALU.is_gt))
        for k in range(2, 8):
            dop(lambda e, k=k: e.scalar_tensor_tensor(out=V32(NSO), in0=V32(TOT), scalar=256.0 * k, in1=V32(NSO),
                                                     op0=ALU.is_gt, op1=ALU.add))
        dop(lambda e: e.tensor_tensor_scan(out=V32(OCI), data0=V32(ONE32), data1=V32(NSO), initial=0.0, op0=ALU.mult, op1=ALU.add))
        dop(lambda e: e.tensor_tensor(out=V32(OST), in0=V32(OCI), in1=V32(NSO), op=ALU.subtract))
        dop(lambda e: e.tensor_scalar(out=V32(DELTA), in0=V32(OST), scalar1=256.0, scalar2=float(32 * 256 - 256),
                                      op0=ALU.mult, op1=ALU.add))
        dop(lambda e: e.tensor_tensor(out=V32(DELTA), in0=V32(DELTA), in1=V32(BASEA), op=ALU.subtract))
        dop(lambda e: e.tensor_scalar(out=F3(TMP), in0=F3(POS), scalar1=256.0, scalar2=None, op0=ALU.is_ge))
        dop(lambda e: e.tensor_tensor(out=F3(TMP), in0=F3(TMP), in1=bc(V32(DELTA), [128, NT, 32], 1), op=ALU.mult))
        dop(lambda e: e.tensor_tensor(out=F3(DEST), in0=F3(POS), in1=bc(V32(BASEA), [128, NT, 32], 1), op=ALU.add))
        dop(lambda e: e.tensor_tensor(out=F3(DEST), in0=F3(DEST), in1=F3(TMP), op=ALU.add))
        dop(lambda e: e.tensor_tensor(out=F3(TMP), in0=F3(DEST), in1=F3(M321), op=ALU.mult))
        dop(lambda e: e.tensor_reduce(out=V16(DR1), in_=F3(TMP), axis=AX.X, op=ALU.add))
        dop(lambda e: e.tensor_tensor(out=F3(TMP), in0=F3(DEST), in1=F3(M322), op=ALU.mult))
        dop(lambda e: e.tensor_reduce(out=V16(DR2), in_=F3(TMP), axis=AX.X, op=ALU.add))
        dve.op(lambda e: e.tensor_copy(out=IDX1[:, 0:NT], in_=V16(DR1)), reads=[rr], writes=[R("idx")])
        dve.op(lambda e: e.tensor_copy(out=IDX2[:, 0:NT], in_=V16(DR2)), reads=[rr], writes=[R("idx")])
        dop(lambda e: e.tensor_tensor(out=F3(TMP), in0=bc(V32(OCI), [128, NT, 32], 1), in1=bc(V16(OIDX), [128, NT, 32], 2),
                                      op=ALU.is_le))
        dop(lambda e: e.tensor_reduce(out=V16(EIDO), in_=F3(TMP), axis=AX.X, op=ALU.add))
        if not OOB_SKIP:
            dop(lambda e: e.tensor_scalar(out=V16(EIDO), in0=V16(EIDO), scalar1=31.0, scalar2=None, op0=ALU.min))
        for h in range(2):
            dop(lambda e, h=h: e.tensor_scalar(out=IW[:, h * 16:(h + 1) * 16], in0=V16(EIDO), scalar1=256.0, scalar2=float(h), op0=ALU.mult,
                                               op1=ALU.add))
            dop(lambda e, h=h: e.scalar_tensor_tensor(out=IW[:, h * 16:(h + 1) * 16], in0=PIDX[:, 0:1].to_broadcast([128, NT]), scalar=2.0,
                                                      in1=IW[:, h * 16:(h + 1) * 16], op0=ALU.mult, op1=ALU.add))
        dve.op(lambda e: e.tensor_copy(out=IDXW, in_=IW), reads=[rr], writes=[R("idxw")])

        for t in range(NT):
            for (idx, nm) in ((IDX1, "1"), (IDX2, "2")):
                K.scatter(XS_D, idx[:, t:t + 1].bitcast(U32), XNALL[:, t, :], reads=[rXN[t], R("idx"), R("xs_zero")],
                          writes=[R("xs_d")])
        K.barrier()

        ki = [0]
        gsi = [0]
        rpl = [R(f"pl{b_}") for b_ in range(4)]
        rph = [R(f"ph{b_}") for b_ in range(4)]

        cast_steps = []

        def issue_weights(q_):
            s_ = ORDER[q_]
            wb = q_ % 3
            if s_ < 32:
                load_slot_weights_static(q_)
                return
            o = s_ - 32
            parts = [(wt, wd, nm, h) for (wt, wd, nm) in ((WG, w_eg_d, "wg"), (WU, w_eu_d, "wu"), (WD, w_ed_d, "wd")) for h in range(2)]

            def gather_part(k):
                wt, wd, nm, h = parts[k]
                K.gather(WST[k % 2], wd, IDXW[:, h * 16 + o:h * 16 + o + 1].bitcast(U32), bound_reg if OOB_SKIP else None,
                         reads=[R("idxw")], writes=[R(f"wst{k % 2}")])

            def cast_part(k):
                wt, wd, nm, h = parts[k]
                wst = WST[k % 2]
                rwst = R(f"wst{k % 2}")
                if k % 2 == 0:
                    dve.op(lambda e: e.tensor_copy(out=wt[wb][:, h * 2048:(h + 1) * 2048], in_=wst), reads=[rwst],
                           writes=[R(f"{nm}{wb}")])
                else:
                    act.op(lambda e: e.activation(out=wt[wb][:, h * 2048:(h + 1) * 2048], in_=wst, func=AF.Copy), reads=[rwst],
                           writes=[R(f"{nm}{wb}")])
                if k + 2 < 6:
                    gather_part(k + 2)
            gather_part(0)
            gather_part(1)
            for k in range(6):
                cast_steps.append(lambda k=k: cast_part(k))

        def tick_casts():
            if cast_steps:
                cast_steps.pop(0)()

        def issue_xsg(q_):
            s_ = ORDER[q_]
            K.dma(sp, XSG[q_ % 2], XS_D[s_ * 256:(s_ + 1) * 256, :].rearrange("(j p) d -> p j d", p=128), reads=[R("xs_d")],
                  writes=[R(f"xsg{q_ % 2}")])

        def emit_transposes(s_):
            xsg = XSG[s_ % 2]
            xst = XST[s_ % 2]
            for j in range(2):
                tb = 6 + j
                psb = ps[tb][:].bitcast(BF16)

                def tr(e, j=j, psb=psb, xsg=xsg):
                    last = None
                    for c in range(NCH):
                        last = e.transpose(psb[:, c * 128:(c + 1) * 128], xsg[:, j, c * 128:(c + 1) * 128], IDENT)
                    return last
                pe_group(tr, [R(f"xsg{s_ % 2}"), R("ident")], [rps[tb]])
                act.op(lambda e, j=j, psb=psb, xst=xst: e.activation(out=xst[:, :, j * 128:(j + 1) * 128],
                                                                     in_=psb.rearrange("p (c s) -> p c s", c=NCH), func=AF.Copy),
                       reads=[rps[tb]], writes=[R(f"xst{s_ % 2}")])

        def emit_gu(s_):
            wb = s_ % 3
            wg = WG[wb].rearrange("p (c f) -> p c f", c=NCH)
            wu = WU[wb].rearrange("p (c f) -> p c f", c=NCH)
            xst = XST[s_ % 2]
            rxst = R(f"xst{s_ % 2}")
            at = AT[s_ % 2]
            for fc in range(4):
                bg = gsi[0] % 2
                bu = 2 + gsi[0] % 2
                sg = SG[gsi[0] % 2]
                rsg = R(f"sg{gsi[0] % 2}")
                gsi[0] += 1

                def emit_g(e, fc=fc, wg=wg, xst=xst, bg=bg):
                    last = None
                    for c in range(NCH):
                        last = e.matmul(ps[bg][:, 0:256], lhsT=wg[:, c, fc * 128:(fc + 1) * 128], rhs=xst[:, c, :],
                                        start=(c == 0), stop=(c == NCH - 1))
                    return last
                pe_group(emit_g, [R(f"wg{wb}"), rxst], [rps[bg]])

                def emit_u(e, fc=fc, wu=wu, xst=xst, bu=bu):
                    last = None
                    for c in range(NCH):
                        last = e.matmul(ps[bu][:, 0:256], lhsT=wu[:, c, fc * 128:(fc + 1) * 128], rhs=xst[:, c, :],
                                        start=(c == 0), stop=(c == NCH - 1))
                    return last
                pe_group(emit_u, [R(f"wu{wb}"), rxst], [rps[bu]])
                act.op(lambda e, sg=sg, bg=bg: e.activation(out=sg, in_=ps[bg][:, 0:256], func=AF.Silu), reads=[rps[bg]], writes=[rsg])
                dve.op(lambda e, sg=sg, bu=bu, fc=fc, at=at: e.tensor_tensor(out=at[:, fc, :], in0=sg, in1=ps[bu][:, 0:256], op=ALU.mult),
                       reads=[rsg, rps[bu]], writes=[R(f"at{s_ % 2}")])
                tick_casts()

        def emit_y(q_):
            s_ = q_
            wb = q_ % 3
            YSB = YSBS[q_ % 2]
            rysb = R(f"ysb{q_ % 2}")
            wd_ = WD[wb].rearrange("p (c d) -> p c d", c=4)
            at = AT[s_ % 2]
            for j in range(2):
                for hf in range(2):
                    by = 4 + (2 * j + hf) % 2

                    def emit_y_(e, j=j, hf=hf, by=by, at=at, wd_=wd_):
                        last = None
                        for fc in range(4):
                            last = e.matmul(ps[by][:, :], lhsT=at[:, fc, j * 128:(j + 1) * 128],
                                            rhs=wd_[:, fc, hf * 512:(hf + 1) * 512], start=(fc == 0), stop=(fc == 3))
                        return last
                    pe_group(emit_y_, [R(f"wd{wb}"), R(f"at{s_ % 2}")], [rps[by]])
                    if hf == 0:
                        act.op(lambda e, j=j, hf=hf, by=by: e.activation(out=YSB[:, j, hf * 512:(hf + 1) * 512], in_=ps[by][:, :],
                                                                         func=AF.Copy),
                               reads=[rps[by]], writes=[rysb])
                    else:
                        dve.op(lambda e, j=j, hf=hf, by=by: e.tensor_copy(out=YSB[:, j, hf * 512:(hf + 1) * 512], in_=ps[by][:, :]),
                               reads=[rps[by]], writes=[rysb])
                    tick_casts()
            sd = ORDER[q_]
            K.dma(sp, YS_D[sd * 256:(sd + 1) * 256, :].rearrange("(j p) d -> p j d", p=128), YSB, reads=[rysb],
                  writes=[R("ys_d")], merge=True)

        for k_ in range(2):
            dve.op(lambda e, k_=k_: e.memset(WST[k_], 0.0), writes=[R(f"wst{k_}")])
        issue_weights(2)
        issue_xsg(0)
        issue_xsg(1)
        emit_transposes(0)
        cc = Carver(UNION_OFF, ARENA_BYTES)
        WPG = cc(NCH * D * 2, BF16, "p (c d) -> p c d", c=NCH)
        WPP = cc(2 * D * 2, BF16, "p (c d) -> p c d", c=2)
        PALL = cc(NT * PLE * 2, BF16, "p (t f) -> p t f", t=NT)
        assert (NSLOT - 1) % 2 == 0
        pre_toks = []
        w_pg_v = w_pg_d.rearrange("(c p) d -> p c d", p=128)
        for s_ in range(NSLOT):
            emit_gu(s_)
            if s_ + 1 < NSLOT:
                emit_transposes(s_ + 1)
            if s_ + 2 < NSLOT:
                issue_xsg(s_ + 2)
            if s_ == NSLOT - 1:
                pre_toks.append(K.dma(pool, WPG[:, 0:4, :], w_pg_v[:, 0:4, :], writes=[R("wpg0"), R("xsg0"), R("xsg1")]))
                pre_toks.append(K.dma(pool, WPG[:, 4:8, :], w_pg_v[:, 4:8, :], writes=[R("wpg1"), R("xst0"), R("xst1")]))
            emit_y(s_)
            while cast_steps:
                tick_casts()
            if s_ + 3 < NSLOT:
                issue_weights(s_ + 3)
        pre_toks.append(K.dma(pool, WPP, w_pp_d.rearrange("(c p) d -> p c d", p=128), writes=[R("wpp"), R("at0"), R("at1")]))
        pre_toks.append(K.dma(pool, PALL, p_d.rearrange("(t p) f -> p t f", p=128),
                              writes=[R("pall"), R("sg0"), R("sg1"), R("ysb0")]))
        K.barrier(skip=set(pre_toks))

        cy = Carver(R2_OFF, UNION_OFF)
        YG = [cy(D * 4, F32) for _ in range(4)]
        PTT = [cc(PLE * 2, BF16, "p (c s) -> p c s", c=2) for _ in range(2)]
        GATE = [cc(512 * 4, F32) for _ in range(2)]
        TMPC = [cc(512 * 4, F32) for _ in range(2)]
        OUTB = [cc(D * 4, F32) for _ in range(2)]
        def load_ple_weights():
            pass
        def combine(t):
            for k_, (idx, wv) in enumerate(((IDX1, W1), (IDX2, W2))):
                yg = YG[(2 * t + k_) % 4]
                ryg = R(f"yg{(2 * t + k_) % 4}")
                K.gather(yg, YS_D, idx[:, t:t + 1].bitcast(U32), None, reads=[R("idx"), R("ys_d")], writes=[ryg])
                dve.op(lambda e, yg=yg, wv=wv: e.scalar_tensor_tensor(out=H[:, t, :], in0=yg, scalar=wv[:, t:t + 1], in1=H[:, t, :],
                                                                     op0=ALU.mult, op1=ALU.add),
                       reads=[ryg, R("w12"), rH[t]], writes=[rH[t]])

        if stage == 2:
            load_ple_weights()
            for t in range(NT):
                combine(t)
            dump_H_and_finish()
            return nc

        ci = [0]

        def ple_tile(t):
            pb = PALL[:, t, :]
            rpb = R("pall")
            ptt = PTT[t % 2]
            rptt = R(f"ptt{t % 2}")
            tb = 5
            psb = ps[tb][:].bitcast(BF16)

            def tr(e):
                e.transpose(psb[:, 0:128], pb[:, 0:128], IDENT)
                return e.transpose(psb[:, 128:256], pb[:, 128:256], IDENT)
            pe_group(tr, [rpb, R("ident")], [rps[tb]])
            act.op(lambda e: e.activation(out=ptt, in_=psb[:, 0:256].rearrange("p (c s) -> p c s", c=2), func=AF.Copy),
                   reads=[rps[tb]], writes=[rptt])
            for hf in range(2):
                bgt = ci[0] % 2
                bpj = 2 + ci[0] % 2
                gt = GATE[ci[0] % 2]
                rgt = R(f"gate{ci[0] % 2}")
                tc_ = TMPC[ci[0] % 2]
                rtc = R(f"tmpc{ci[0] % 2}")
                ci[0] += 1

                def emit_gt(e):
                    last = None
                    for c in range(NCH):
                        last = e.matmul(ps[bgt][:, :], lhsT=XT[:, c, t * 128:(t + 1) * 128],
                                        rhs=WPG[:, c, hf * 512:(hf + 1) * 512], start=(c == 0), stop=(c == NCH - 1))
                    return last
                pe_group(emit_gt, [R("wpg0"), R("wpg1"), rXT[t // 4]], [rps[bgt]])

                def emit_pj(e):
                    e.matmul(ps[bpj][:, :], lhsT=ptt[:, 0, :], rhs=WPP[:, 0, hf * 512:(hf + 1) * 512], start=True, stop=False)
                    return e.matmul(ps[bpj][:, :], lhsT=ptt[:, 1, :], rhs=WPP[:, 1, hf * 512:(hf + 1) * 512], start=False,
                                    stop=True)
                pe_group(emit_pj, [R("wpp"), rptt], [rps[bpj]])
                act.op(lambda e: e.activation(out=gt, in_=ps[bgt][:, :], func=AF.Sigmoid), reads=[rps[bgt]], writes=[rgt])
                dve.op(lambda e: e.tensor_tensor(out=tc_, in0=gt, in1=ps[bpj][:, :], op=ALU.mult), reads=[rgt, rps[bpj]], writes=[rtc])
                dve.op(lambda e: e.tensor_tensor(out=H[:, t, hf * 512:(hf + 1) * 512], in0=tc_, in1=H[:, t, hf * 512:(hf + 1) * 512],
                                                 op=ALU.add),
                       reads=[rtc, rH[t]], writes=[rH[t]])

        toks = []

        def final_tile(t):
            ob_ = OUTB[t % 2]
            rob = R(f"outb{t % 2}")
            dve.op(lambda e: e.scalar_tensor_tensor(out=ob_, in0=H[:, t, :], scalar=RSS[1][:, t:t + 1], in1=GG[1],
                                                    op0=ALU.mult, op1=ALU.mult),
                   reads=[rH[t], R(f"rs1_{t // 4}"), R("G1")], writes=[rob])
            toks.append(K.dma(sp, out_d[t * 128:(t + 1) * 128, :], ob_, reads=[rob], writes=[R("out")], merge=True))

        load_gain(g_ple_d, 0)
        load_gain(g_fin_d, 1)
        for step in range(6):
            g1 = step - 1
            if 0 <= g1 < 4:
                stats_group(0, g1)
                norm_tiles(0, g1)
            if step < 4:
                for t in range(4 * step, 4 * step + 4):
                    combine(t)
                if step == 0:
                    load_ple_weights()
            if 0 <= g1 < 4:
                for t in range(4 * g1, 4 * g1 + 4):
                    ple_tile(t)
            g2 = step - 2
            if 0 <= g2 < 4:
                stats_group(1, g2)
                for t in range(4 * g2, 4 * g2 + 4):
                    final_tile(t)
        for s_, v_ in toks:
            sp.wait_tok(s_, v_)
    return nc


def make_in_maps(inputs):
    f = lambda a: np.ascontiguousarray(np.asarray(a, dtype=np.float32))
    x = f(inputs["x"])
    p = f(inputs["p"])[0]
    shared = {
        "g_mix": f(inputs["g_mix"]).reshape(1, D),
        "w_in": f(inputs["w_in"])[0],
        "b_f": f(inputs["b_f"]).reshape(8, 1),
        "w_pool": f(np.transpose(np.asarray(inputs["w_pool"])[0], (1, 0, 2))),
        "s_pool": f(np.asarray(inputs["s_pool"]).reshape(4, 128).T),
        "w_out": f(inputs["w_out"])[0],
        "g_ffn": f(inputs["g_ffn"]).reshape(1, D),
        "w_r": f(np.concatenate([np.asarray(inputs["w_grp"])[0], np.asarray(inputs["w_rt"])[0]], axis=1)),
        "b_r": f(np.concatenate([np.asarray(inputs["b_grp"])[0], np.asarray(inputs["b_rt"])[0]], axis=0)).reshape(1, 36),
        "w_e_gate": f(np.asarray(inputs["w_e_gate"]).reshape(NEXP, 8, 128, 512).transpose(0, 2, 1, 3)).reshape(NEXP * 256, 2048),
        "w_e_up": f(np.asarray(inputs["w_e_up"]).reshape(NEXP, 8, 128, 512).transpose(0, 2, 1, 3)).reshape(NEXP * 256, 2048),
        "w_e_down": f(np.asarray(inputs["w_e_down"]).reshape(NEXP, 4, 128, D).transpose(0, 2, 1, 3)).reshape(NEXP * 256, 2048),
        "g_ple": f(inputs["g_ple"]).reshape(1, D),
        "w_ple_gate": f(inputs["w_ple_gate"])[0],
        "w_ple_proj": f(inputs["w_ple_proj"])[0],
        "g_final": f(inputs["g_final"]).reshape(1, D),
    }
    maps = []
    for b in range(N_CORES):
        m = dict(shared)
        m["x"] = np.ascontiguousarray(x[b])
        m["p"] = np.ascontiguousarray(p[b])
        maps.append(m)
    return maps


def kernel(**inputs):
    nc = build_program()
    in_maps = make_in_maps(inputs)
    res = run_bass_kernel_spmd(nc, in_maps, core_ids=list(range(N_CORES)))
    return np.stack([np.asarray(r["out"], dtype=np.float32) for r in res.results], axis=0)
```

```python
import numpy as np
from contextlib import ExitStack
import concourse.bass as bass
import concourse.mybir as mybir
from concourse.bass_utils import run_bass_kernel_spmd

F32 = mybir.dt.float32
BF16 = mybir.dt.bfloat16
I32 = mybir.dt.int32
U32 = mybir.dt.uint32
AF = mybir.ActivationFunctionType
ALU = mybir.AluOpType
AX = mybir.AxisListType

S = 2048
D = 1024
NT = 16
NCH = 8
PLE = 256
EPS = 1e-6
N_CORES = 8
NEXP = 32
NOVF = 15
NSLOT = 32 + NOVF
NPRE = 28
PE_SKIP_SELF = True
OOB_SKIP = True
ARENA_BYTES = 210944


class Res:
    __slots__ = ("name", "w", "r")

    def __init__(self, name):
        self.name = name
        self.w = {}
        self.r = {}


class Eng:
    def __init__(self, name, obj, sem):
        self.name = name
        self.e = obj
        self.sem = sem
        self.cnt = 0
        self.waited = {}

    def wait_tok(self, s, v):
        if v > 0 and self.waited.get(s, 0) < v:
            self.e.wait_ge(s, v)
            self.waited[s] = v

    def sync(self, reads=(), writes=(), merge_ok=None):
        deps = {}

        def add(s, v):
            if deps.get(s, 0) < v:
                deps[s] = v
        for r in reads:
            for s, v in r.w.items():
                add(s, v)
        for w in writes:
            for s, v in w.w.items():
                if merge_ok is None or s not in merge_ok:
                    add(s, v)
            for s, v in w.r.items():
                add(s, v)
        for s, v in deps.items():
            if s is self.sem and self.name == "pe" and PE_SKIP_SELF:
                continue
            self.wait_tok(s, v)

    def done(self, ins, reads=(), writes=()):
        self.cnt += 1
        ins.then_inc(self.sem, 1)
        tok = (self.sem, self.cnt)
        for r in reads:
            if r.r.get(self.sem, 0) < self.cnt:
                r.r[self.sem] = self.cnt
        for w in writes:
            w.w = {self.sem: self.cnt}
            w.r = {}
        return tok

    def op(self, fn, reads=(), writes=()):
        self.sync(reads, writes)
        ins = fn(self.e)
        return self.done(ins, reads, writes)


class Kern:
    def __init__(self, nc, stack, n_hw=24, n_sw=24):
        self.nc = nc
        mk = lambda n: stack.enter_context(nc.semaphore(n))
        self.pe = Eng("pe", nc.tensor, mk("s_pe"))
        self.act = Eng("act", nc.scalar, mk("s_act"))
        self.dve = Eng("dve", nc.vector, mk("s_dve"))
        self.pool = Eng("pool", nc.gpsimd, mk("s_pool"))
        self.sp = Eng("sp", nc.sync, mk("s_sp"))
        self.engs = [self.pe, self.act, self.dve, self.pool, self.sp]
        self.hw = [[mk(f"s_hw{i}"), 0] for i in range(n_hw)]
        self.sw = [[mk(f"s_sw{i}"), 0] for i in range(n_sw)]
        self.hwi = 0
        self.swi = 0
        self.dma_sems = set(x[0] for x in self.hw) | set(x[0] for x in self.sw)

    def _slot(self, eng):
        if eng is self.pool:
            slot = self.sw[self.swi]
            self.swi = (self.swi + 1) % len(self.sw)
        else:
            slot = self.hw[self.hwi]
            self.hwi = (self.hwi + 1) % len(self.hw)
        return slot

    def _pre(self, eng, reads, writes, merge):
        slot = self._slot(eng)
        sem, cnt = slot
        eng.sync(reads, writes, merge_ok=self.dma_sems if merge else None)
        eng.wait_tok(sem, cnt)
        slot[1] = cnt + 16
        return sem, cnt + 16

    def _post(self, ins, sem, val, reads, writes, merge):
        ins.then_inc(sem, 16)
        for r in reads:
            if r.r.get(sem, 0) < val:
                r.r[sem] = val
        for w in writes:
            if merge:
                w.w[sem] = val
            else:
                w.w = {sem: val}
                w.r = {}
        return (sem, val)

    def dma(self, eng, out, in_, reads=(), writes=(), merge=False):
        sem, val = self._pre(eng, reads, writes, merge)
        ins = eng.e.dma_start(out=out, in_=in_)
        return self._post(ins, sem, val, reads, writes, merge)

    def scatter(self, out_dram, idx, in_sb, reads=(), writes=(), merge=True):
        sem, val = self._pre(self.pool, reads, writes, merge)
        ins = self.pool.e.indirect_dma_start(out=out_dram, out_offset=bass.IndirectOffsetOnAxis(ap=idx, axis=0),
                                             in_=in_sb, in_offset=None)
        return self._post(ins, sem, val, reads, writes, merge)

    def gather(self, out_sb, in_dram, idx, bound=None, reads=(), writes=(), merge=False):
        sem, val = self._pre(self.pool, reads, writes, merge)
        off = bass.IndirectOffsetOnAxis(ap=idx, axis=0)
        if bound is None:
            ins = self.pool.e.indirect_dma_start(out=out_sb, out_offset=None, in_=in_dram, in_offset=off)
        else:
            ins = self.pool.e.indirect_dma_start(out=out_sb, out_offset=None, in_=in_dram, in_offset=off,
                                                 bounds_check=bound, oob_is_err=False)
        return self._post(ins, sem, val, reads, writes, merge)

    def barrier(self, skip=()):
        for e in self.engs:
            for x in self.engs:
                if x is not e:
                    e.wait_tok(x.sem, x.cnt)
            for sem, cnt in self.hw + self.sw:
                if (sem, cnt) in skip:
                    continue
                e.wait_tok(sem, cnt)


def build_program(stage=99):
    nc = bass.Bass("TRN2", target_bir_lowering=False)
    dram = lambda n, shp, kind="ExternalInput": nc.dram_tensor(n, shp, F32, kind=kind).ap()
    x_d = dram("x", [S, D])
    p_d = dram("p", [S, PLE])
    g_mix_d = dram("g_mix", [1, D])
    w_in_d = dram("w_in", [D, 2056])
    b_f_d = dram("b_f", [8, 1])
    w_pool_d = dram("w_pool", [128, 4, 128])
    s_pool_d = dram("s_pool", [128, 4])
    w_out_d = dram("w_out", [D, D])
    g_ffn_d = dram("g_ffn", [1, D])
    w_r_d = dram("w_r", [D, 36])
    b_r_d = dram("b_r", [1, 36])
    w_eg_d = dram("w_e_gate", [NEXP * 256, 2048])
    w_eu_d = dram("w_e_up", [NEXP * 256, 2048])
    w_ed_d = dram("w_e_down", [NEXP * 256, 2048])
    g_ple_d = dram("g_ple", [1, D])
    w_pg_d = dram("w_ple_gate", [D, D])
    w_pp_d = dram("w_ple_proj", [PLE, D])
    g_fin_d = dram("g_final", [1, D])
    out_d = dram("out", [S, D], kind="ExternalOutput")

    with ExitStack() as st:
        K = Kern(nc, st)
        pe, act, dve, pool, sp = K.pe, K.act, K.dve, K.pool, K.sp
        arena = nc.alloc_sbuf_tensor("arena", [128, ARENA_BYTES // 4], F32)

        def view(off, nbytes, dt, pat=None, **kw):
            assert off % 4 == 0 and nbytes % 4 == 0 and off + nbytes <= ARENA_BYTES, (off, nbytes)
            a = arena[:, off // 4:(off + nbytes) // 4]
            if dt != F32:
                a = a.bitcast(dt)
            if pat:
                a = a.rearrange(pat, **kw)
            return a

        class Carver:
            def __init__(self, base, limit):
                self.o = base
                self.limit = limit

            def __call__(self, nbytes, dt, pat=None, **kw):
                nb = (nbytes + 63) // 64 * 64
                v = view(self.o, nb, dt)
                if nb != nbytes:
                    v = v[:, 0:nbytes // (2 if dt == BF16 else 4)]
                if pat:
                    v = v.rearrange(pat, **kw)
                self.o += nb
                assert self.o <= self.limit, (self.o, self.limit)
                return v

        _res = {}

        def R(name):
            if name not in _res:
                _res[name] = Res(name)
            return _res[name]

        ps = [st.enter_context(nc.psum_tensor(f"ps{i}", [128, 512], F32)) for i in range(8)]
        rps = [R(f"ps{i}") for i in range(8)]

        cp = Carver(0, ARENA_BYTES)
        H_OFF = cp.o
        H = cp(NT * D * 4, F32, "p (t d) -> p t d", t=NT)
        XT_OFF = cp.o
        XT = cp(NCH * S * 2, BF16, "p (c s) -> p c s", c=NCH)
        IDENT = cp(256, BF16)
        IDENTF = cp(512, F32)
        MASK = cp(256, BF16)
        MASKF = cp(512, F32)
        GG = [cp(D * 4, F32) for _ in range(2)]
        SSS = [cp(64, F32) for _ in range(2)]
        RSS = [cp(64, F32) for _ in range(2)]
        EPSC = cp(64, F32)
        INVC = cp(64, F32)
        ONEC = cp(64, F32)
        XN = [cp(D * 2, BF16) for _ in range(2)]
        JUNK = cp(D * 2, BF16)
        R2_OFF = cp.o
        rH = [R(f"H{t}") for t in range(NT)]
        rXT = [R(f"XT{b}") for b in range(4)]

        pool.op(lambda e: e.memset(IDENTF, 0.0), writes=[R("identf")])
        pool.op(lambda e: e.affine_select(out=IDENTF, in_=IDENTF, pattern=[[-1, 128]], compare_op=ALU.not_equal,
                                          fill=1.0, base=0, channel_multiplier=1),
                reads=[R("identf")], writes=[R("identf")])
        dve.op(lambda e: e.tensor_copy(out=IDENT, in_=IDENTF), reads=[R("identf")], writes=[R("ident")])
        pool.op(lambda e: e.memset(MASKF, 0.0), writes=[R("maskf")])
        pool.op(lambda e: e.affine_select(out=MASKF, in_=MASKF, pattern=[[1, 128]], compare_op=ALU.is_ge,
                                          fill=-30000.0, base=0, channel_multiplier=-1),
                reads=[R("maskf")], writes=[R("maskf")])
        dve.op(lambda e: e.tensor_copy(out=MASK, in_=MASKF), reads=[R("maskf")], writes=[R("mask")])
        dve.op(lambda e: e.memset(EPSC, EPS), writes=[R("epsc")])
        dve.op(lambda e: e.memset(ONEC, 1.0), writes=[R("onec")])
        for i in range(16):
            dve.op(lambda e, i=i: e.memset(INVC[:, i:i + 1], 1.0 / (i + 1)), writes=[R("invc")])

        bank_rr = [0]

        def next_bank(lo=0, hi=8):
            b = lo + bank_rr[0] % (hi - lo)
            bank_rr[0] += 1
            return b

        def pe_group(emit, reads, writes):
            pe.sync(reads, writes)
            last = emit(pe.e)
            return pe.done(last, reads, writes)

        def load_x(step=4):
            xv = x_d.rearrange("(t p) d -> p t d", p=128)
            for i in range(0, NT, step):
                K.dma(sp, H[:, i:i + step, :], xv[:, i:i + step, :], writes=rH[i:i + step])

        def load_gain(g_dram, k):
            K.dma(sp, GG[k], g_dram[0:1, :].partition_broadcast(128), writes=[R(f"G{k}")])

        def stats_group(k, g):
            for t in range(4 * g, 4 * g + 4):
                act.op(lambda e, t=t: e.activation(out=JUNK, in_=H[:, t, :], func=AF.Square, accum_out=SSS[k][:, t:t + 1]),
                       reads=[rH[t]], writes=[R(f"ss{k}_{g}")])
            act.op(lambda e: e.activation(out=RSS[k][:, 4 * g:4 * g + 4], in_=SSS[k][:, 4 * g:4 * g + 4], func=AF.Sqrt,
                                          scale=1.0 / D, bias=EPSC[:, 0:1]),
                   reads=[R(f"ss{k}_{g}"), R("epsc")], writes=[R(f"rs{k}_{g}")])
            dve.op(lambda e: e.reciprocal(out=RSS[k][:, 4 * g:4 * g + 4], in_=RSS[k][:, 4 * g:4 * g + 4]),
                   reads=[R(f"rs{k}_{g}")], writes=[R(f"rs{k}_{g}")])

        def stats(g_dram, k):
            load_gain(g_dram, k)
            for g in range(4):
                stats_group(k, g)

        def norm_tiles(k, g):
            for t in range(4 * g, 4 * g + 4):
                xn = XN[t % 2]
                rxn = R(f"xn{t % 2}")
                dve.op(lambda e, t=t, xn=xn: e.scalar_tensor_tensor(out=xn, in0=H[:, t, :], scalar=RSS[k][:, t:t + 1], in1=GG[k],
                                                                   op0=ALU.mult, op1=ALU.mult),
                       reads=[rH[t], R(f"rs{k}_{t // 4}"), R(f"G{k}")], writes=[rxn])
                b = 6 + t % 2
                psb = ps[b][:].bitcast(BF16)

                def tr(e, xn=xn, psb=psb):
                    last = None
                    for c in range(NCH):
                        last = e.transpose(psb[:, c * 128:(c + 1) * 128], xn[:, c * 128:(c + 1) * 128], IDENT)
                    return last
                pe_group(tr, [rxn, R("ident")], [rps[b]])
                act.op(lambda e, t=t, psb=psb: e.activation(out=XT[:, :, t * 128:(t + 1) * 128],
                                                            in_=psb.rearrange("p (c s) -> p c s", c=NCH), func=AF.Copy),
                       reads=[rps[b]], writes=[rXT[t // 4]])

        def norm_to_XT(g_dram, k):
            load_gain(g_dram, k)
            stats_group(k, 0)
            for g in range(4):
                if g + 1 < 4:
                    stats_group(k, g + 1)
                norm_tiles(k, g)

        def dump_H_and_finish():
            toks = []
            ov = out_d.rearrange("(t p) d -> p t d", p=128)
            for i in range(4):
                toks.append(K.dma(sp, ov[:, 4 * i:4 * i + 4, :], H[:, 4 * i:4 * i + 4, :], reads=rH[4 * i:4 * i + 4],
                                  writes=[R("out")]))
            for s_, v_ in toks:
                sp.wait_tok(s_, v_)

        load_x()
        c2_ = Carver(R2_OFF, ARENA_BYTES)
        WB = [c2_(NCH * 512 * 2, BF16, "p (c f) -> p c f", c=NCH) for _ in range(2)]
        WBF = c2_(NCH * 8 * 2, BF16, "p (c f) -> p c f", c=NCH)
        w_in_v = w_in_d.rearrange("(c p) f -> p c f", p=128)
        K.dma(pool, WBF, w_in_v[:, :, 2048:2056], writes=[R("wbf")])
        K.dma(pool, WB[0], w_in_v[:, :, 0:512], writes=[R("wb0")])
        K.dma(pool, WB[1], w_in_v[:, :, 512:1024], writes=[R("wb1")])
        ZT = view(ARENA_BYTES - D * 2, D * 2, BF16)
        dve.op(lambda e: e.memset(ZT, 0.0), writes=[R("zt")])
        XS_D = nc.dram_tensor("xs_d", [NSLOT * 256, D], BF16).ap()
        YS_D = nc.dram_tensor("ys_d", [NSLOT * 256, D], F32).ap()
        norm_to_XT(g_mix_d, 0)
        K.barrier()

        c1 = Carver(H_OFF, H_OFF + NT * D * 4)
        QT = c1(4 * S * 2, BF16, "p (c s) -> p c s", c=4)
        KT = c1(4 * S * 2, BF16, "p (c s) -> p c s", c=4)
        AUGQ_T = [c1(S * 2, BF16) for _ in range(3)]
        AUGK_T = [c1(S * 2, BF16) for _ in range(3)]
        AUGQ = [AUGQ_T[h // 3][32 * (h % 3):32 * (h % 3) + 6, :] for h in range(8)]
        AUGK = [AUGK_T[h // 3][32 * (h % 3):32 * (h % 3) + 6, :] for h in range(8)]
        PT = [c1(512 * 2, BF16) for _ in range(4)]
        OTOK = [c1(4 * 128 * 2, BF16, "p (i f) -> p i f", i=4) for _ in range(2)]
        REC = [c1(64, F32) for _ in range(2)]
        FIX = c1(64, F32)

        MIXTLO = c2_(4 * S * 2, BF16, "p (c s) -> p c s", c=4)
        PW_OFF = c2_.o
        U = c2_(S * 4, F32)
        PA = c2_(S * 4, F32)
        PB = c2_(S * 4, F32)
        DT = c2_(S * 2, BF16)
        PW_END = c2_.o
        RQ3 = c2_(3 * S * 2, BF16, "p (i s) -> p i s", i=3)
        V = c2_(NT * 8 * 65 * 2, BF16, "p (t h e) -> p t h e", t=NT, h=8)
        WP = c2_(4 * 128 * 2, BF16, "p (g d) -> p g d", g=4)
        SPOOL = c2_(64, F32)
        NBF = c2_(64, F32)

        K.dma(pool, WP, w_pool_d[:, :, :], writes=[R("wp")])
        K.dma(sp, SPOOL[:, 0:4], s_pool_d[:, :], writes=[R("spool")])
        K.dma(sp, NBF[0:8, 0:1], b_f_d[:, :], writes=[R("nbf")])
        dve.op(lambda e: e.tensor_scalar(out=NBF[0:8, 0:1], in0=NBF[0:8, 0:1], scalar1=-1.0, scalar2=None, op0=ALU.mult),
               reads=[R("nbf")], writes=[R("nbf")])

        def proj_fm(wb, rwb, col0, ncol, sb, bank, nrows=128):
            def emit(e):
                last = None
                for c in range(NCH):
                    last = e.matmul(ps[bank][0:nrows, :], lhsT=wb[:, c, col0:col0 + ncol],
                                    rhs=XT[:, c, sb * 512:(sb + 1) * 512], start=(c == 0), stop=(c == NCH - 1))
                return last
            pe_group(emit, [rwb, rXT[sb]], [rps[bank]])

        for sb in range(4):
            b = next_bank()
            proj_fm(WBF, R("wbf"), 0, 8, sb, b, nrows=8)
            act.op(lambda e, sb=sb, b=b: e.activation(out=PA[0:8, sb * 512:(sb + 1) * 512], in_=ps[b][0:8, :], func=AF.Exp,
                                                      scale=-1.0, bias=NBF[0:8, 0:1]),
                   reads=[rps[b], R("nbf")], writes=[R("PA")])
        act.op(lambda e: e.activation(out=PA[0:8, :], in_=PA[0:8, :], func=AF.Ln, scale=1.0, bias=ONEC[0:8, 0:1]),
               reads=[R("PA"), R("onec")], writes=[R("PA")])
        dve.op(lambda e: e.memset(U[0:8, :], 1.0), writes=[R("U")])
        dve.op(lambda e: e.tensor_tensor_scan(out=PB[0:8, :], data0=U[0:8, :], data1=PA[0:8, :], initial=0.0,
                                              op0=ALU.mult, op1=ALU.add),
               reads=[R("U"), R("PA")], writes=[R("PB")])
        dve.op(lambda e: e.tensor_scalar(out=RQ3[0:8, 0, :], in0=PB[0:8, :], scalar1=-1.0, scalar2=None, op0=ALU.mult),
               reads=[R("PB")], writes=[R("rq3")])
        dve.op(lambda e: e.scalar_tensor_tensor(out=PA[0:8, :], in0=PB[0:8, :], scalar=-1.0, in1=RQ3[0:8, 0, :],
                                                op0=ALU.mult, op1=ALU.subtract),
               reads=[R("PB"), R("rq3")], writes=[R("PA")])
        dve.op(lambda e: e.tensor_copy(out=RQ3[0:8, 1, :], in_=PA[0:8, :]), reads=[R("PA")], writes=[R("rq3")])
        dve.op(lambda e: e.tensor_tensor(out=PA[0:8, :], in0=PA[0:8, :], in1=RQ3[0:8, 1, :], op=ALU.subtract),
               reads=[R("PA"), R("rq3")], writes=[R("PA")])
        dve.op(lambda e: e.tensor_copy(out=RQ3[0:8, 2, :], in_=PA[0:8, :]), reads=[R("PA")], writes=[R("rq3")])

        for i in range(3):
            hs = [h for h in range(8) if h // 3 == i]
            pool.op(lambda e, i=i: e.memset(AUGQ_T[i], -1.0), writes=[R(f"augq{h}") for h in hs])
            pool.op(lambda e, i=i: e.memset(AUGK_T[i], 1.0), writes=[R(f"augk{h}") for h in hs])
        for h in range(8):
            for i in range(3):
                K.dma(sp, AUGQ[h][i:i + 1, :], RQ3[h:h + 1, i, :], reads=[R("rq3")], writes=[R(f"augq{h}")], merge=True)
                K.dma(sp, AUGK[h][3 + i:4 + i, :], RQ3[h:h + 1, i, :], reads=[R("rq3")], writes=[R(f"augk{h}")], merge=True)

        for s_ in range(NSLOT):
            K.dma(sp, XS_D[s_ * 256:(s_ + 1) * 256, :].rearrange("(j p) d -> p j d", p=128),
                  ZT.unsqueeze(1).to_broadcast([128, 2, D]), reads=[R("zt")], writes=[R("xs_zero")], merge=True)

        for g in range(4):
            for sb in range(4):
                b = next_bank()
                proj_fm(WB[0], R("wb0"), g * 128, 128, sb, b)
                act.op(lambda e, sb=sb, b=b: e.activation(out=U[:, sb * 512:(sb + 1) * 512], in_=ps[b][:, :], func=AF.Copy),
                       reads=[rps[b]], writes=[R("U")])
            w = 2 ** (g + 1)
            src, rsrc = U, R("U")
            bufs = [(PA, R("PA")), (PB, R("PB"))]
            for k in range(g + 1):
                sh = 2 ** k
                dst, rdst = bufs[k % 2]
                dve.op(lambda e, src=src, dst=dst, sh=sh: e.tensor_tensor(out=dst[:, sh:S], in0=src[:, sh:S], in1=src[:, 0:S - sh],
                                                                         op=ALU.add),
                       reads=[rsrc], writes=[rdst])
                dve.op(lambda e, src=src, dst=dst, sh=sh: e.tensor_copy(out=dst[:, 0:sh], in_=src[:, 0:sh]),
                       reads=[rsrc], writes=[rdst])
                src, rsrc = dst, rdst
            dve.op(lambda e, src=src, w=w: e.scalar_tensor_tensor(out=DT, in0=src, scalar=1.0 / w, in1=U, op0=ALU.mult,
                                                                 op1=ALU.subtract),
                   reads=[rsrc, R("U")], writes=[R("DT")])
            dve.op(lambda e, src=src, w=w: e.tensor_tensor(out=FIX[:, 0:w - 1], in0=src[:, 0:w - 1], in1=INVC[:, 0:w - 1],
                                                          op=ALU.mult),
                   reads=[rsrc, R("invc")], writes=[R("fix")])
            dve.op(lambda e, w=w: e.tensor_tensor(out=DT[:, 0:w - 1], in0=FIX[:, 0:w - 1], in1=U[:, 0:w - 1], op=ALU.subtract),
                   reads=[R("fix"), R("U")], writes=[R("DT")])
            c2 = g
            for sb in range(4):
                b = next_bank()
                proj_fm(WB[1], R("wb1"), c2 * 128, 128, sb, b)
                act.op(lambda e, c2=c2, sb=sb, b=b: e.activation(out=QT[:, c2, sb * 512:(sb + 1) * 512], in_=ps[b][:, :],
                                                                 func=AF.Copy, scale=0.125),
                       reads=[rps[b]], writes=[R(f"qt{c2}")])
            for sb in range(4):
                b = next_bank()
                pe_group(lambda e, g=g, sb=sb, b=b: e.matmul(ps[b][:, :], lhsT=WP[:, g, :], rhs=DT[:, sb * 512:(sb + 1) * 512],
                                                             start=True, stop=True),
                         [R("wp"), R("DT")], [rps[b]])
                act.op(lambda e, g=g, sb=sb, b=b: e.activation(out=MIXTLO[:, g, sb * 512:(sb + 1) * 512], in_=ps[b][:, :],
                                                               func=AF.Copy, scale=SPOOL[:, g:g + 1]),
                       reads=[rps[b], R("spool")], writes=[R("mixtlo")])

        K.dma(pool, WB[0], w_in_v[:, :, 1024:1536], writes=[R("wb0")])
        K.dma(pool, WB[1], w_in_v[:, :, 1536:2048], writes=[R("wb1")])
        WO = view(PW_OFF, NCH * D * 2, BF16, "p (c d) -> p c d", c=NCH)
        assert PW_OFF + NCH * D * 2 <= PW_END
        w_out_v = w_out_d.rearrange("(c p) d -> p c d", p=128)
        K.dma(pool, WO[:, 0:4, :], w_out_v[:, 0:4, :], writes=[R("wo0"), R("U")])
        K.dma(pool, WO[:, 4:8, :], w_out_v[:, 4:8, :], writes=[R("wo1"), R("PA")])
        WBF_D = {nm: nc.dram_tensor(f"wbf_{nm}", [max(NPRE, 1) * 256, 2048], BF16).ap() for nm in ("wg", "wu", "wd")}
        for e_ in range(NPRE):
            for (nm, wd) in (("wg", w_eg_d), ("wu", w_eu_d), ("wd", w_ed_d)):
                K.dma(pool, WBF_D[nm][e_ * 256:(e_ + 1) * 256, :].rearrange("(p h) f -> p h f", h=2),
                      wd[e_ * 256:(e_ + 1) * 256, :].rearrange("(p h) f -> p h f", h=2), writes=[R(f"wbf_{nm}{e_}")])
        for c2 in range(4):
            for sb in range(4):
                b = next_bank()
                proj_fm(WB[0], R("wb0"), c2 * 128, 128, sb, b)
                dve.op(lambda e, c2=c2, sb=sb, b=b: e.tensor_copy(out=KT[:, c2, sb * 512:(sb + 1) * 512], in_=ps[b][:, :]),
                       reads=[rps[b]], writes=[R(f"kt{c2}")])
        dve.op(lambda e: e.memset(V[:, :, :, 64:65], 1.0), writes=[R("V")])
        for t in range(NT):
            b = next_bank()

            def emit(e, t=t, b=b):
                last = None
                for c in range(NCH):
                    last = e.matmul(ps[b][:, :], lhsT=XT[:, c, t * 128:(t + 1) * 128], rhs=WB[1][:, c, :],
                                    start=(c == 0), stop=(c == NCH - 1))
                return last
            pe_group(emit, [R("wb1"), rXT[t // 4]], [rps[b]])
            src_v = ps[b][:, :].rearrange("p (h e) -> p h e", h=8)
            if t % 2 == 0:
                act.op(lambda e, t=t, src_v=src_v: e.activation(out=V[:, t, :, 0:64], in_=src_v, func=AF.Copy),
                       reads=[rps[b]], writes=[R("V")])
            else:
                dve.op(lambda e, t=t, src_v=src_v: e.tensor_copy(out=V[:, t, :, 0:64], in_=src_v),
                       reads=[rps[b]], writes=[R("V")])

        LA = 2
        items = []
        gidx = 0
        for c2 in range(4):
            for qb in range(4):
                for half in range(2):
                    nj = 4 * qb + 4
                    for j in range(nj):
                        items.append((c2, qb, half, j, gidx, j == nj - 1))
                    gidx += 1
        from collections import deque
        pend = deque()
        deferred = []
        cnt_s = [0]

        def emit_S(it):
            c2, qb, half, j, g, last = it
            hd = 2 * c2 + half
            base = 64 * half
            q_lo = max(qb * 512, j * 128)
            n = (qb + 1) * 512 - q_lo
            k = cnt_s[0]
            cnt_s[0] += 1
            sbk = 2 + k % 4
            pt = PT[k % 4]
            rpt = R(f"pt{k % 4}")
            diag = j >= 4 * qb

            def emit_s(e):
                e.matmul(ps[sbk][:, 0:n], lhsT=KT[base:base + 64, c2, j * 128:(j + 1) * 128],
                         rhs=QT[base:base + 64, c2, q_lo:q_lo + n], start=True, stop=False)
                last_ = e.matmul(ps[sbk][:, 0:n], lhsT=AUGK[hd][:, j * 128:(j + 1) * 128],
                                 rhs=AUGQ[hd][:, q_lo:q_lo + n], start=False, stop=not diag)
                if diag:
                    last_ = e.matmul(ps[sbk][:, 0:128], lhsT=IDENT, rhs=MASK, start=False, stop=True)
                return last_
            pe_group(emit_s, [R(f"kt{c2}"), R(f"qt{c2}"), R(f"augk{hd}"), R(f"augq{hd}"), R("mask"), R("ident")], [rps[sbk]])
            act.op(lambda e: e.activation(out=pt[:, 0:n], in_=ps[sbk][:, 0:n], func=AF.Exp), reads=[rps[sbk]], writes=[rpt])
            return (it, q_lo, pt, rpt)

        def emit_PV(rec):
            (c2, qb, half, j, g, last), q_lo, pt, rpt = rec
            h = 2 * c2 + half
            base = 64 * half
            ob = g % 2
            O = ps[ob][:, 0:260].rearrange("p (i e) -> p i e", e=65)

            def emit_o(e):
                last_ = None
                for i in range(max(j, 4 * qb), 4 * qb + 4):
                    off = i * 128 - q_lo
                    last_ = e.matmul(O[:, i - 4 * qb, :], lhsT=pt[:, off:off + 128], rhs=V[:, j, h, :],
                                     start=(j == 0 and i == 4 * qb), stop=(j == i), skip_group_check=True)
                return last_
            pe_group(emit_o, [rpt, R("V")], [rps[ob]])
            if not last:
                return
            ot = OTOK[qb % 2]
            rot = R(f"otok{qb % 2}")
            rec_ = REC[ob]
            rrec = R(f"rec{ob}")
            dve.op(lambda e: e.reciprocal(out=rec_[:, 0:4], in_=O[:, :, 64]), reads=[rps[ob]], writes=[rrec])
            dve.op(lambda e: e.tensor_tensor(out=ot[:, :, base:base + 64], in0=O[:, :, 0:64],
                                             in1=rec_[:, 0:4].unsqueeze(2).to_broadcast([128, 4, 64]), op=ALU.mult),
                   reads=[rps[ob], rrec], writes=[rot])
            if half == 1:
                def fin():
                    tb = 6 + qb % 2
                    psb = ps[tb][:].bitcast(BF16)

                    def tr(e):
                        last_ = None
                        for i in range(4):
                            last_ = e.transpose(psb[:, i * 128:(i + 1) * 128], ot[:, i, :], IDENT)
                        return last_
                    pe_group(tr, [rot, R("ident")], [rps[tb]])
                    act.op(lambda e: e.activation(out=XT[:, c2, qb * 512:(qb + 1) * 512], in_=psb[:, 0:512], func=AF.Copy),
                           reads=[rps[tb]], writes=[rXT[qb]])
                deferred.append([3, fin])

        def tick():
            for d_ in list(deferred):
                d_[0] -= 1
                if d_[0] <= 0:
                    deferred.remove(d_)
                    d_[1]()

        for it in items:
            pend.append(emit_S(it))
            if len(pend) > LA:
                emit_PV(pend.popleft())
            tick()
        while pend:
            emit_PV(pend.popleft())
            tick()
        while deferred:
            tick()

        K.barrier()
        load_x(step=1)
        for t in range(NT):
            for hf in range(2):
                b = next_bank()

                def emit(e, t=t, hf=hf, b=b):
                    last = None
                    for c in range(NCH):
                        lhsT = MIXTLO[:, c, t * 128:(t + 1) * 128] if c < 4 else XT[:, c - 4, t * 128:(t + 1) * 128]
                        last = e.matmul(ps[b][:, :], lhsT=lhsT, rhs=WO[:, c, hf * 512:(hf + 1) * 512],
                                        start=(c == 0), stop=(c == NCH - 1))
                    return last
                pe_group(emit, [R("mixtlo"), rXT[t // 4], R("wo0"), R("wo1")], [rps[b]])
                dve.op(lambda e, t=t, hf=hf, b=b: e.tensor_tensor(out=H[:, t, hf * 512:(hf + 1) * 512], in0=ps[b][:, :],
                                                                in1=H[:, t, hf * 512:(hf + 1) * 512], op=ALU.add),
                       reads=[rps[b], rH[t]], writes=[rH[t]])
        if stage == 1:
            dump_H_and_finish()
            return nc

        stats(g_ffn_d, 1)
        K.barrier()
        XNALL = XT.rearrange("p c s -> p (c s)").rearrange("p (t d) -> p t d", t=NT)
        rXN = [R(f"xnall{t}") for t in range(NT)]
        cb = Carver(R2_OFF, ARENA_BYTES)
        WG = [cb(4096 * 2, BF16) for _ in range(2)]
        WU = [cb(4096 * 2, BF16) for _ in range(2)]
        WD = [cb(4096 * 2, BF16) for _ in range(2)]
        W1 = cb(64, F32)
        W2 = cb(64, F32)
        IDX1 = cb(64, I32)
        IDX2 = cb(64, I32)
        IDXW = cb(2 * 16 * 4, I32)
        UNION_OFF = cb.o
        cr = Carver(UNION_OFF, ARENA_BYTES)
        XTT = [cr(NCH * 128 * 2, BF16, "p (c s) -> p c s", c=NCH) for _ in range(2)]
        WR = cr(NCH * 36 * 2, BF16, "p (c f) -> p c f", c=NCH)
        B36 = cr(36 * 4, F32)
        L = cr(NT * 36 * 4, F32, "p (t f) -> p t f", t=NT)
        GMAX, GW, M1, M2, ED, DR1, DR2, EIDO, OIDX = [cr(64, F32) for _ in range(9)]
        OH4 = cr(NT * 4 * 4, F32, "p (t g) -> p t g", t=NT)
        E4 = cr(NT * 4 * 4, F32, "p (t g) -> p t g", t=NT)
        SEL, SEL2, MK1, MK2 = [cr(NT * 8 * 4, F32, "p (t e) -> p t e", t=NT) for _ in range(4)]
        TMP, M321, M322, MSUM, POS, DEST = [cr(NT * 32 * 4, F32, "p (t g e) -> p t g e", t=NT, g=4) for _ in range(6)]
        MSUMB = cr(NT * 32 * 2, BF16, "p (t e) -> p t e", t=NT)
        TOT, NSO, OCI, OST, DELTA, BASEA, ONE32 = [cr(128, F32) for _ in range(7)]
        PIDX = cr(64, F32)
        IW = cr(2 * 16 * 4, F32)
        ONESB = cr(256, BF16)
        TRIB = cr(256, BF16)
        TRIF = cr(512, F32)
        cs = Carver(UNION_OFF, ARENA_BYTES)
        XSG = [cs(2 * D * 2, BF16, "p (j d) -> p j d", j=2) for _ in range(2)]
        XST = [cs(NCH * 256 * 2, BF16, "p (c s) -> p c s", c=NCH) for _ in range(2)]
        AT = [cs(4 * 256 * 2, BF16, "p (c s) -> p c s", c=4) for _ in range(2)]
        SG = [cs(256 * 2, BF16) for _ in range(2)]
        YSBS = [cs(2 * D * 4, F32, "p (j d) -> p j d", j=2) for _ in range(2)]
        WST = [cs(2048 * 4, F32)]
        cx = Carver(XT_OFF, XT_OFF + NCH * S * 2)
        WG.append(cx(4096 * 2, BF16))
        WU.append(cx(4096 * 2, BF16))
        WD.append(cx(4096 * 2, BF16))
        WST.append(cx(2048 * 4, F32))


        def wview(wd, e_):
            return wd.rearrange("(e p h) f -> e p h f", p=128, h=2)[e_]

        ORDER = []
        for i_ in range(16):
            ORDER += [2 * i_, 2 * i_ + 1] + ([32 + i_] if i_ < NOVF else [])
        assert sorted(ORDER) == list(range(NSLOT))

        def load_slot_weights_static(q_):
            s_ = ORDER[q_]
            wb = q_ % 3
            for (wt, wd, nm) in ((WG, w_eg_d, "wg"), (WU, w_eu_d, "wu"), (WD, w_ed_d, "wd")):
                if s_ < NPRE:
                    K.dma(pool, wt[wb].rearrange("p (h f) -> p h f", h=2), wview(WBF_D[nm], s_), reads=[R(f"wbf_{nm}{s_}")],
                          writes=[R(f"{nm}{wb}")])
                else:
                    K.dma(pool, wt[wb].rearrange("p (h f) -> p h f", h=2), wview(wd, s_), writes=[R(f"{nm}{wb}")])

        bound_reg = None
        if OOB_SKIP:
            bound_reg = nc.gpsimd.alloc_register("wbound")
            nc.gpsimd.reg_mov(bound_reg, NEXP * 256 - 1)

        dve.op(lambda e: e.memset(ONESB, 1.0), writes=[R("onesb")])
        pool.op(lambda e: e.memset(TRIF, 1.0), writes=[R("trif")])
        pool.op(lambda e: e.affine_select(out=TRIF, in_=TRIF, pattern=[[1, 128]], compare_op=ALU.is_ge, fill=0.0,
                                          base=-1, channel_multiplier=-1), reads=[R("trif")], writes=[R("trif")])
        dve.op(lambda e: e.tensor_copy(out=TRIB, in_=TRIF), reads=[R("trif")], writes=[R("trib")])
        for i in range(32):
            dve.op(lambda e, i=i: e.memset(BASEA[:, i:i + 1], 256.0 * i), writes=[R("basea")])
        for i in range(16):
            dve.op(lambda e, i=i: e.memset(OIDX[:, i:i + 1], float(i)), writes=[R("oidx")])
        dve.op(lambda e: e.memset(ONE32, 1.0), writes=[R("one32")])
        pe_group(lambda e: e.matmul(ps[5][:, 0:1], lhsT=TRIB, rhs=ONESB[:, 0:1], start=True, stop=True),
                 [R("trib"), R("onesb")], [rps[5]])
        dve.op(lambda e: e.tensor_copy(out=PIDX[:, 0:1], in_=ps[5][:, 0:1]), reads=[rps[5]], writes=[R("pidx")])

        K.dma(pool, WR, w_r_d.rearrange("(c p) f -> p c f", p=128), writes=[R("wr")])
        K.dma(sp, B36, b_r_d[0:1, :].partition_broadcast(128), writes=[R("b36")])
        load_slot_weights_static(0)
        load_slot_weights_static(1)

        def router(t):
            xtt = XTT[t % 2]
            rb = 4 + t % 2

            def emit(e):
                last = None
                for c in range(NCH):
                    last = e.matmul(ps[rb][:, 0:36], lhsT=xtt[:, c, :], rhs=WR[:, c, :], start=(c == 0), stop=(c == NCH - 1))
                return last
            pe_group(emit, [R("wr"), R(f"xtt{t % 2}")], [rps[rb]])
            dve.op(lambda e: e.tensor_tensor(out=L[:, t, :], in0=ps[rb][:, 0:36], in1=B36, op=ALU.add),
                   reads=[rps[rb], R("b36")], writes=[R("L")])

        for t in range(NT):
            dve.op(lambda e, t=t: e.scalar_tensor_tensor(out=XNALL[:, t, :], in0=H[:, t, :], scalar=RSS[1][:, t:t + 1], in1=GG[1],
                                                        op0=ALU.mult, op1=ALU.mult),
                   reads=[rH[t], R(f"rs1_{t // 4}"), R("G1")], writes=[rXN[t]])
            b = 6 + t % 2
            psb = ps[b][:].bitcast(BF16)

            def tr(e, t=t, psb=psb):
                last = None
                for c in range(NCH):
                    last = e.transpose(psb[:, c * 128:(c + 1) * 128], XNALL[:, t, c * 128:(c + 1) * 128], IDENT)
                return last
            pe_group(tr, [rXN[t], R("ident")], [rps[b]])
            act.op(lambda e, t=t, psb=psb: e.activation(out=XTT[t % 2], in_=psb.rearrange("p (c s) -> p c s", c=NCH), func=AF.Copy),
                   reads=[rps[b]], writes=[R(f"xtt{t % 2}")])
            if t >= 1:
                router(t - 1)
        router(NT - 1)

        rr = R("route")
        Lg = L[:, :, 0:4]
        Le = L[:, :, 4:36].rearrange("p t (g e) -> p t g e", g=4)
        bc = lambda a, shp, ax: a.unsqueeze(ax).to_broadcast(shp)
        V16 = lambda a: a[:, 0:NT]
        V32 = lambda a: a[:, 0:32]
        F3 = lambda a: a.rearrange("p t g e -> p t (g e)")
        croute = [R("L"), rr, R("basea"), R("oidx"), R("one32"), R("pidx")]
        dop = lambda fn: dve.op(fn, reads=croute, writes=[rr])
        dop(lambda e: e.tensor_reduce(out=V16(GMAX), in_=Lg, axis=AX.X, op=ALU.max))
        dop(lambda e: e.tensor_tensor(out=OH4, in0=Lg, in1=bc(V16(GMAX), [128, NT, 4], 2), op=ALU.is_equal))
        dop(lambda e: e.tensor_tensor(out=E4, in0=Lg, in1=bc(V16(GMAX), [128, NT, 4], 2), op=ALU.subtract))
        act.op(lambda e: e.activation(out=E4, in_=E4, func=AF.Exp), reads=[rr], writes=[rr])
        dop(lambda e: e.tensor_reduce(out=V16(GW), in_=E4, axis=AX.X, op=ALU.add))
        dop(lambda e: e.reciprocal(out=V16(GW), in_=V16(GW)))
        dop(lambda e: e.tensor_tensor(out=TMP, in0=Le, in1=bc(OH4, [128, NT, 4, 8], 3), op=ALU.mult))
        dop(lambda e: e.tensor_reduce(out=SEL, in_=TMP.rearrange("p t g e -> p t e g"), axis=AX.X, op=ALU.add))
        dop(lambda e: e.tensor_reduce(out=V16(M1), in_=SEL, axis=AX.X, op=ALU.max))
        dop(lambda e: e.tensor_tensor(out=MK1, in0=SEL, in1=bc(V16(M1), [128, NT, 8], 2), op=ALU.is_equal))
        dop(lambda e: e.scalar_tensor_tensor(out=SEL2, in0=MK1, scalar=-1e30, in1=SEL, op0=ALU.mult, op1=ALU.add))
        dop(lambda e: e.tensor_reduce(out=V16(M2), in_=SEL2, axis=AX.X, op=ALU.max))
        dop(lambda e: e.tensor_tensor(out=MK2, in0=SEL2, in1=bc(V16(M2), [128, NT, 8], 2), op=ALU.is_equal))
        dop(lambda e: e.tensor_tensor(out=V16(ED), in0=V16(M2), in1=V16(M1), op=ALU.subtract))
        act.op(lambda e: e.activation(out=V16(ED), in_=V16(ED), func=AF.Exp), reads=[rr], writes=[rr])
        dve.op(lambda e: e.tensor_scalar(out=V16(W1), in0=V16(ED), scalar1=1.0, scalar2=None, op0=ALU.add),
               reads=[rr], writes=[R("w12")])
        dve.op(lambda e: e.reciprocal(out=V16(W1), in_=V16(W1)), reads=[R("w12")], writes=[R("w12")])
        dve.op(lambda e: e.tensor_tensor(out=V16(W1), in0=V16(W1), in1=V16(GW), op=ALU.mult), reads=[R("w12"), rr], writes=[R("w12")])
        dve.op(lambda e: e.tensor_tensor(out=V16(W2), in0=V16(ED), in1=V16(W1), op=ALU.mult), reads=[R("w12"), rr], writes=[R("w12")])
        dop(lambda e: e.tensor_tensor(out=M321, in0=bc(OH4, [128, NT, 4, 8], 3), in1=bc(MK1, [128, NT, 4, 8], 2), op=ALU.mult))
        dop(lambda e: e.tensor_tensor(out=M322, in0=bc(OH4, [128, NT, 4, 8], 3), in1=bc(MK2, [128, NT, 4, 8], 2), op=ALU.mult))
        dop(lambda e: e.tensor_tensor(out=MSUM, in0=M321, in1=M322, op=ALU.add))
        dop(lambda e: e.tensor_copy(out=MSUMB, in_=F3(MSUM)))
        for t in range(NT):
            pb_ = 4 + t % 2

            def emit(e, t=t, pb_=pb_):
                for t2 in range(t):
                    e.matmul(ps[pb_][:, 0:32], lhsT=ONESB, rhs=MSUMB[:, t2, :], start=(t2 == 0), stop=False)
                return e.matmul(ps[pb_][:, 0:32], lhsT=TRIB, rhs=MSUMB[:, t, :], start=(t == 0), stop=True)
            pe_group(emit, [rr, R("onesb"), R("trib")], [rps[pb_]])
            dve.op(lambda e, t=t, pb_=pb_: e.tensor_copy(out=F3(POS)[:, t, :], in_=ps[pb_][:, 0:32]), reads=[rps[pb_]], writes=[R("pos")])

        def emit(e):
            last = None
            for t in range(NT):
                last = e.matmul(ps[5][:, 0:32], lhsT=ONESB, rhs=MSUMB[:, t, :], start=(t == 0), stop=(t == NT - 1))
            return last
        pe_group(emit, [rr, R("onesb")], [rps[5]])
        dve.op(lambda e: e.tensor_copy(out=V32(TOT), in_=ps[5][:, 0:32]), reads=[rps[5]], writes=[rr])
        croute.append(R("pos"))
        dop(lambda e: e.tensor_scalar(out=V32(NSO), in0=V32(TOT), scalar1=256.0, scalar2=None, op0=ALU.is_gt))
        for k in range(2, 8):
            dop(lambda e, k=k: e.scalar_tensor_tensor(out=V32(NSO), in0=V32(TOT), scalar=256.0 * k, in1=V32(NSO),
                                                     op0=ALU.is_gt, op1=ALU.add))
        dop(lambda e: e.tensor_tensor_scan(out=V32(OCI), data0=V32(ONE32), data1=V32(NSO), initial=0.0, op0=ALU.mult, op1=ALU.add))
        dop(lambda e: e.tensor_tensor(out=V32(OST), in0=V32(OCI), in1=V32(NSO), op=ALU.subtract))
        dop(lambda e: e.tensor_scalar(out=V32(DELTA), in0=V32(OST), scalar1=256.0, scalar2=float(32 * 256 - 256),
                                      op0=ALU.mult, op1=ALU.add))
        dop(lambda e: e.tensor_tensor(out=V32(DELTA), in0=V32(DELTA), in1=V32(BASEA), op=ALU.subtract))
        dop(lambda e: e.tensor_scalar(out=F3(TMP), in0=F3(POS), scalar1=256.0, scalar2=None, op0=ALU.is_ge))
        dop(lambda e: e.tensor_tensor(out=F3(TMP), in0=F3(TMP), in1=bc(V32(DELTA), [128, NT, 32], 1), op=ALU.mult))
        dop(lambda e: e.tensor_tensor(out=F3(DEST), in0=F3(POS), in1=bc(V32(BASEA), [128, NT, 32], 1), op=ALU.add))
        dop(lambda e: e.tensor_tensor(out=F3(DEST), in0=F3(DEST), in1=F3(TMP), op=ALU.add))
        dop(lambda e: e.tensor_tensor(out=F3(TMP), in0=F3(DEST), in1=F3(M321), op=ALU.mult))
        dop(lambda e: e.tensor_reduce(out=V16(DR1), in_=F3(TMP), axis=AX.X, op=ALU.add))
        dop(lambda e: e.tensor_tensor(out=F3(TMP), in0=F3(DEST), in1=F3(M322), op=ALU.mult))
        dop(lambda e: e.tensor_reduce(out=V16(DR2), in_=F3(TMP), axis=AX.X, op=ALU.add))
        dve.op(lambda e: e.tensor_copy(out=IDX1[:, 0:NT], in_=V16(DR1)), reads=[rr], writes=[R("idx")])
        dve.op(lambda e: e.tensor_copy(out=IDX2[:, 0:NT], in_=V16(DR2)), reads=[rr], writes=[R("idx")])
        dop(lambda e: e.tensor_tensor(out=F3(TMP), in0=bc(V32(OCI), [128, NT, 32], 1), in1=bc(V16(OIDX), [128, NT, 32], 2),
                                      op=ALU.is_le))
        dop(lambda e: e.tensor_reduce(out=V16(EIDO), in_=F3(TMP), axis=AX.X, op=ALU.add))
        if not OOB_SKIP:
            dop(lambda e: e.tensor_scalar(out=V16(EIDO), in0=V16(EIDO), scalar1=31.0, scalar2=None, op0=ALU.min))
        for h in range(2):
            dop(lambda e, h=h: e.tensor_scalar(out=IW[:, h * 16:(h + 1) * 16], in0=V16(EIDO), scalar1=256.0, scalar2=float(h), op0=ALU.mult,
                                               op1=ALU.add))
            dop(lambda e, h=h: e.scalar_tensor_tensor(out=IW[:, h * 16:(h + 1) * 16], in0=PIDX[:, 0:1].to_broadcast([128, NT]), scalar=2.0,
                                                      in1=IW[:, h * 16:(h + 1) * 16], op0=ALU.mult, op1=ALU.add))
        dve.op(lambda e: e.tensor_copy(out=IDXW, in_=IW), reads=[rr], writes=[R("idxw")])

        for t in range(NT):
            for (idx, nm) in ((IDX1, "1"), (IDX2, "2")):
                K.scatter(XS_D, idx[:, t:t + 1].bitcast(U32), XNALL[:, t, :], reads=[rXN[t], R("idx"), R("xs_zero")],
                          writes=[R("xs_d")])
        K.barrier()

        ki = [0]
        gsi = [0]
        rpl = [R(f"pl{b_}") for b_ in range(4)]
        rph = [R(f"ph{b_}") for b_ in range(4)]

        cast_steps = []

        def issue_weights(q_):
            s_ = ORDER[q_]
            wb = q_ % 3
            if s_ < 32:
                load_slot_weights_static(q_)
                return
            o = s_ - 32
            parts = [(wt, wd, nm, h) for (wt, wd, nm) in ((WG, w_eg_d, "wg"), (WU, w_eu_d, "wu"), (WD, w_ed_d, "wd")) for h in range(2)]

            def gather_part(k):
                wt, wd, nm, h = parts[k]
                K.gather(WST[k % 2], wd, IDXW[:, h * 16 + o:h * 16 + o + 1].bitcast(U32), bound_reg if OOB_SKIP else None,
                         reads=[R("idxw")], writes=[R(f"wst{k % 2}")])

            def cast_part(k):
                wt, wd, nm, h = parts[k]
                wst = WST[k % 2]
                rwst = R(f"wst{k % 2}")
                if k % 2 == 0:
                    dve.op(lambda e: e.tensor_copy(out=wt[wb][:, h * 2048:(h + 1) * 2048], in_=wst), reads=[rwst],
                           writes=[R(f"{nm}{wb}")])
                else:
                    act.op(lambda e: e.activation(out=wt[wb][:, h * 2048:(h + 1) * 2048], in_=wst, func=AF.Copy), reads=[rwst],
                           writes=[R(f"{nm}{wb}")])
                if k + 2 < 6:
                    gather_part(k + 2)
            gather_part(0)
            gather_part(1)
            for k in range(6):
                cast_steps.append(lambda k=k: cast_part(k))

        def tick_casts():
            if cast_steps:
                cast_steps.pop(0)()

        def issue_xsg(q_):
            s_ = ORDER[q_]
            K.dma(sp, XSG[q_ % 2], XS_D[s_ * 256:(s_ + 1) * 256, :].rearrange("(j p) d -> p j d", p=128), reads=[R("xs_d")],
                  writes=[R(f"xsg{q_ % 2}")])

        def emit_transposes(s_):
            xsg = XSG[s_ % 2]
            xst = XST[s_ % 2]
            for j in range(2):
                tb = 6 + j
                psb = ps[tb][:].bitcast(BF16)

                def tr(e, j=j, psb=psb, xsg=xsg):
                    last = None
                    for c in range(NCH):
                        last = e.transpose(psb[:, c * 128:(c + 1) * 128], xsg[:, j, c * 128:(c + 1) * 128], IDENT)
                    return last
                pe_group(tr, [R(f"xsg{s_ % 2}"), R("ident")], [rps[tb]])
                act.op(lambda e, j=j, psb=psb, xst=xst: e.activation(out=xst[:, :, j * 128:(j + 1) * 128],
                                                                     in_=psb.rearrange("p (c s) -> p c s", c=NCH), func=AF.Copy),
                       reads=[rps[tb]], writes=[R(f"xst{s_ % 2}")])

        def emit_gu(s_):
            wb = s_ % 3
            wg = WG[wb].rearrange("p (c f) -> p c f", c=NCH)
            wu = WU[wb].rearrange("p (c f) -> p c f", c=NCH)
            xst = XST[s_ % 2]
            rxst = R(f"xst{s_ % 2}")
            at = AT[s_ % 2]
            for fc in range(4):
                bg = gsi[0] % 2
                bu = 2 + gsi[0] % 2
                sg = SG[gsi[0] % 2]
                rsg = R(f"sg{gsi[0] % 2}")
                gsi[0] += 1

                def emit_g(e, fc=fc, wg=wg, xst=xst, bg=bg):
                    last = None
                    for c in range(NCH):
                        last = e.matmul(ps[bg][:, 0:256], lhsT=wg[:, c, fc * 128:(fc + 1) * 128], rhs=xst[:, c, :],
                                        start=(c == 0), stop=(c == NCH - 1))
                    return last
                pe_group(emit_g, [R(f"wg{wb}"), rxst], [rps[bg]])

                def emit_u(e, fc=fc, wu=wu, xst=xst, bu=bu):
                    last = None
                    for c in range(NCH):
                        last = e.matmul(ps[bu][:, 0:256], lhsT=wu[:, c, fc * 128:(fc + 1) * 128], rhs=xst[:, c, :],
                                        start=(c == 0), stop=(c == NCH - 1))
                    return last
                pe_group(emit_u, [R(f"wu{wb}"), rxst], [rps[bu]])
                act.op(lambda e, sg=sg, bg=bg: e.activation(out=sg, in_=ps[bg][:, 0:256], func=AF.Silu), reads=[rps[bg]], writes=[rsg])
                dve.op(lambda e, sg=sg, bu=bu, fc=fc, at=at: e.tensor_tensor(out=at[:, fc, :], in0=sg, in1=ps[bu][:, 0:256], op=ALU.mult),
                       reads=[rsg, rps[bu]], writes=[R(f"at{s_ % 2}")])
                tick_casts()

        def emit_y(q_):
            s_ = q_
            wb = q_ % 3
            YSB = YSBS[q_ % 2]
            rysb = R(f"ysb{q_ % 2}")
            wd_ = WD[wb].rearrange("p (c d) -> p c d", c=4)
            at = AT[s_ % 2]
            for j in range(2):
                for hf in range(2):
                    by = 4 + (2 * j + hf) % 2

                    def emit_y_(e, j=j, hf=hf, by=by, at=at, wd_=wd_):
                        last = None
                        for fc in range(4):
                            last = e.matmul(ps[by][:, :], lhsT=at[:, fc, j * 128:(j + 1) * 128],
                                            rhs=wd_[:, fc, hf * 512:(hf + 1) * 512], start=(fc == 0), stop=(fc == 3))
                        return last
                    pe_group(emit_y_, [R(f"wd{wb}"), R(f"at{s_ % 2}")], [rps[by]])
                    if hf == 0:
                        act.op(lambda e, j=j, hf=hf, by=by: e.activation(out=YSB[:, j, hf * 512:(hf + 1) * 512], in_=ps[by][:, :],
                                                                         func=AF.Copy),
                               reads=[rps[by]], writes=[rysb])
                    else:
                        dve.op(lambda e, j=j, hf=hf, by=by: e.tensor_copy(out=YSB[:, j, hf * 512:(hf + 1) * 512], in_=ps[by][:, :]),
                               reads=[rps[by]], writes=[rysb])
                    tick_casts()
            sd = ORDER[q_]
            K.dma(sp, YS_D[sd * 256:(sd + 1) * 256, :].rearrange("(j p) d -> p j d", p=128), YSB, reads=[rysb],
                  writes=[R("ys_d")], merge=True)

        for k_ in range(2):
            dve.op(lambda e, k_=k_: e.memset(WST[k_], 0.0), writes=[R(f"wst{k_}")])
        issue_weights(2)
        issue_xsg(0)
        issue_xsg(1)
        emit_transposes(0)
        cc = Carver(UNION_OFF, ARENA_BYTES)
        WPG = cc(NCH * D * 2, BF16, "p (c d) -> p c d", c=NCH)
        WPP = cc(2 * D * 2, BF16, "p (c d) -> p c d", c=2)
        PALL = cc(NT * PLE * 2, BF16, "p (t f) -> p t f", t=NT)
        assert (NSLOT - 1) % 2 == 0
        pre_toks = []
        w_pg_v = w_pg_d.rearrange("(c p) d -> p c d", p=128)
        for s_ in range(NSLOT):
            emit_gu(s_)
            if s_ + 1 < NSLOT:
                emit_transposes(s_ + 1)
            if s_ + 2 < NSLOT:
                issue_xsg(s_ + 2)
            if s_ == NSLOT - 1:
                pre_toks.append(K.dma(pool, WPG[:, 0:4, :], w_pg_v[:, 0:4, :], writes=[R("wpg0"), R("xsg0"), R("xsg1")]))
                pre_toks.append(K.dma(pool, WPG[:, 4:8, :], w_pg_v[:, 4:8, :], writes=[R("wpg1"), R("xst0"), R("xst1")]))
            emit_y(s_)
            while cast_steps:
                tick_casts()
            if s_ + 3 < NSLOT:
                issue_weights(s_ + 3)
        pre_toks.append(K.dma(pool, WPP, w_pp_d.rearrange("(c p) d -> p c d", p=128), writes=[R("wpp"), R("at0"), R("at1")]))
        pre_toks.append(K.dma(pool, PALL, p_d.rearrange("(t p) f -> p t f", p=128),
                              writes=[R("pall"), R("sg0"), R("sg1"), R("ysb0")]))
        K.barrier(skip=set(pre_toks))

        cy = Carver(R2_OFF, UNION_OFF)
        YG = [cy(D * 4, F32) for _ in range(4)]
        PTT = [cc(PLE * 2, BF16, "p (c s) -> p c s", c=2) for _ in range(2)]
        GATE = [cc(512 * 4, F32) for _ in range(2)]
        TMPC = [cc(512 * 4, F32) for _ in range(2)]
        OUTB = [cc(D * 4, F32) for _ in range(2)]
        def load_ple_weights():
            pass
        def combine(t):
            for k_, (idx, wv) in enumerate(((IDX1, W1), (IDX2, W2))):
                yg = YG[(2 * t + k_) % 4]
                ryg = R(f"yg{(2 * t + k_) % 4}")
                K.gather(yg, YS_D, idx[:, t:t + 1].bitcast(U32), None, reads=[R("idx"), R("ys_d")], writes=[ryg])
                dve.op(lambda e, yg=yg, wv=wv: e.scalar_tensor_tensor(out=H[:, t, :], in0=yg, scalar=wv[:, t:t + 1], in1=H[:, t, :],
                                                                     op0=ALU.mult, op1=ALU.add),
                       reads=[ryg, R("w12"), rH[t]], writes=[rH[t]])

        if stage == 2:
            load_ple_weights()
            for t in range(NT):
                combine(t)
            dump_H_and_finish()
            return nc

        ci = [0]

        def ple_tile(t):
            pb = PALL[:, t, :]
            rpb = R("pall")
            ptt = PTT[t % 2]
            rptt = R(f"ptt{t % 2}")
            tb = 5
            psb = ps[tb][:].bitcast(BF16)

            def tr(e):
                e.transpose(psb[:, 0:128], pb[:, 0:128], IDENT)
                return e.transpose(psb[:, 128:256], pb[:, 128:256], IDENT)
            pe_group(tr, [rpb, R("ident")], [rps[tb]])
            act.op(lambda e: e.activation(out=ptt, in_=psb[:, 0:256].rearrange("p (c s) -> p c s", c=2), func=AF.Copy),
                   reads=[rps[tb]], writes=[rptt])
            for hf in range(2):
                bgt = ci[0] % 2
                bpj = 2 + ci[0] % 2
                gt = GATE[ci[0] % 2]
                rgt = R(f"gate{ci[0] % 2}")
                tc_ = TMPC[ci[0] % 2]
                rtc = R(f"tmpc{ci[0] % 2}")
                ci[0] += 1

                def emit_gt(e):
                    last = None
                    for c in range(NCH):
                        last = e.matmul(ps[bgt][:, :], lhsT=XT[:, c, t * 128:(t + 1) * 128],
                                        rhs=WPG[:, c, hf * 512:(hf + 1) * 512], start=(c == 0), stop=(c == NCH - 1))
                    return last
                pe_group(emit_gt, [R("wpg0"), R("wpg1"), rXT[t // 4]], [rps[bgt]])

                def emit_pj(e):
                    e.matmul(ps[bpj][:, :], lhsT=ptt[:, 0, :], rhs=WPP[:, 0, hf * 512:(hf + 1) * 512], start=True, stop=False)
                    return e.matmul(ps[bpj][:, :], lhsT=ptt[:, 1, :], rhs=WPP[:, 1, hf * 512:(hf + 1) * 512], start=False,
                                    stop=True)
                pe_group(emit_pj, [R("wpp"), rptt], [rps[bpj]])
                act.op(lambda e: e.activation(out=gt, in_=ps[bgt][:, :], func=AF.Sigmoid), reads=[rps[bgt]], writes=[rgt])
                dve.op(lambda e: e.tensor_tensor(out=tc_, in0=gt, in1=ps[bpj][:, :], op=ALU.mult), reads=[rgt, rps[bpj]], writes=[rtc])
                dve.op(lambda e: e.tensor_tensor(out=H[:, t, hf * 512:(hf + 1) * 512], in0=tc_, in1=H[:, t, hf * 512:(hf + 1) * 512],
                                                 op=ALU.add),
                       reads=[rtc, rH[t]], writes=[rH[t]])

        toks = []

        def final_tile(t):
            ob_ = OUTB[t % 2]
            rob = R(f"outb{t % 2}")
            dve.op(lambda e: e.scalar_tensor_tensor(out=ob_, in0=H[:, t, :], scalar=RSS[1][:, t:t + 1], in1=GG[1],
                                                    op0=ALU.mult, op1=ALU.mult),
                   reads=[rH[t], R(f"rs1_{t // 4}"), R("G1")], writes=[rob])
            toks.append(K.dma(sp, out_d[t * 128:(t + 1) * 128, :], ob_, reads=[rob], writes=[R("out")], merge=True))

        load_gain(g_ple_d, 0)
        load_gain(g_fin_d, 1)
        for step in range(6):
            g1 = step - 1
            if 0 <= g1 < 4:
                stats_group(0, g1)
                norm_tiles(0, g1)
            if step < 4:
                for t in range(4 * step, 4 * step + 4):
                    combine(t)
                if step == 0:
                    load_ple_weights()
            if 0 <= g1 < 4:
                for t in range(4 * g1, 4 * g1 + 4):
                    ple_tile(t)
            g2 = step - 2
            if 0 <= g2 < 4:
                stats_group(1, g2)
                for t in range(4 * g2, 4 * g2 + 4):
                    final_tile(t)
        for s_, v_ in toks:
            sp.wait_tok(s_, v_)
    return nc


def make_in_maps(inputs):
    f = lambda a: np.ascontiguousarray(np.asarray(a, dtype=np.float32))
    x = f(inputs["x"])
    p = f(inputs["p"])[0]
    shared = {
        "g_mix": f(inputs["g_mix"]).reshape(1, D),
        "w_in": f(inputs["w_in"])[0],
        "b_f": f(inputs["b_f"]).reshape(8, 1),
        "w_pool": f(np.transpose(np.asarray(inputs["w_pool"])[0], (1, 0, 2))),
        "s_pool": f(np.asarray(inputs["s_pool"]).reshape(4, 128).T),
        "w_out": f(inputs["w_out"])[0],
        "g_ffn": f(inputs["g_ffn"]).reshape(1, D),
        "w_r": f(np.concatenate([np.asarray(inputs["w_grp"])[0], np.asarray(inputs["w_rt"])[0]], axis=1)),
        "b_r": f(np.concatenate([np.asarray(inputs["b_grp"])[0], np.asarray(inputs["b_rt"])[0]], axis=0)).reshape(1, 36),
        "w_e_gate": f(np.asarray(inputs["w_e_gate"]).reshape(NEXP, 8, 128, 512).transpose(0, 2, 1, 3)).reshape(NEXP * 256, 2048),
        "w_e_up": f(np.asarray(inputs["w_e_up"]).reshape(NEXP, 8, 128, 512).transpose(0, 2, 1, 3)).reshape(NEXP * 256, 2048),
        "w_e_down": f(np.asarray(inputs["w_e_down"]).reshape(NEXP, 4, 128, D).transpose(0, 2, 1, 3)).reshape(NEXP * 256, 2048),
        "g_ple": f(inputs["g_ple"]).reshape(1, D),
        "w_ple_gate": f(inputs["w_ple_gate"])[0],
        "w_ple_proj": f(inputs["w_ple_proj"])[0],
        "g_final": f(inputs["g_final"]).reshape(1, D),
    }
    maps = []
    for b in range(N_CORES):
        m = dict(shared)
        m["x"] = np.ascontiguousarray(x[b])
        m["p"] = np.ascontiguousarray(p[b])
        maps.append(m)
    return maps


def kernel(**inputs):
    nc = build_program()
    in_maps = make_in_maps(inputs)
    res = run_bass_kernel_spmd(nc, in_maps, core_ids=list(range(N_CORES)))
    return np.stack([np.asarray(r["out"], dtype=np.float32) for r in res.results], axis=0)
```
